# Optimizing a Trainium2 kernel written in Bass

```python
import jax, jax.numpy as jnp
from jax import lax
import numpy as np

D_MODEL = 1024
BATCH = 8
SEQ = 4096
DEPTH = 2

N_MIXERS = 2
D_FF = 2816
D_PLE = 256
CONV_WIDTH = 3
RET_HEADS = 4
RET_QK_DIM = D_MODEL // RET_HEADS
RET_V_DIM = 2 * RET_QK_DIM
RET_CHUNK = 128
ROPE_BASE = 10000.0
NORM_EPS = 1e-6
N_CONV_LAYERS = (DEPTH + N_MIXERS - 1) // N_MIXERS
N_RET_LAYERS = DEPTH // N_MIXERS

N_NORMS = 8
FFN1_PRE, FFN1_POST, MIX_PRE, MIX_POST, FFN2_PRE, FFN2_POST, PLE_PRE, PLE_POST = range(N_NORMS)

kernel_name = "hybrid_shortconv_retention_macaron"


def rmsnorm(x, g):
    xf = x.astype(jnp.float32)
    y = xf * lax.rsqrt(jnp.mean(xf * xf, axis=-1, keepdims=True) + NORM_EPS)
    return (y * g.astype(jnp.float32)).astype(x.dtype)


def swiglu(x, w_gate, w_up, w_down):
    return (jax.nn.silu(x @ w_gate) * (x @ w_up)) @ w_down


def short_conv_mixer(x, w_in, conv_w, w_out):
    T = x.shape[1]
    b, c, h = jnp.split(x @ w_in, 3, axis=-1)
    u = c * h
    u_pad = jnp.pad(u, ((0, 0), (CONV_WIDTH - 1, 0), (0, 0)))
    v = conv_w[0] * u_pad[:, 0:T]
    for k in range(1, CONV_WIDTH):
        v = v + conv_w[k] * u_pad[:, k:k + T]
    return (b * v) @ w_out


def rotary(x, pos):
    half = x.shape[-1] // 2
    inv_freq = 1.0 / (ROPE_BASE ** jnp.linspace(0.0, 1.0, half, dtype=jnp.float32))
    ang = pos.astype(jnp.float32)[:, None] * inv_freq[None, :]
    cos = jnp.cos(ang)[:, None, :]
    sin = jnp.sin(ang)[:, None, :]
    x1, x2 = x[..., :half], x[..., half:]
    return jnp.concatenate([x1 * cos - x2 * sin, x2 * cos + x1 * sin], axis=-1)


def retention_mixer(x, w_in, w_out):
    Bsz, T, _ = x.shape
    H, dk, dv, C = RET_HEADS, RET_QK_DIM, RET_V_DIM, RET_CHUNK
    n_chunks = T // C
    f32 = jnp.float32
    proj = x @ w_in
    q, k, v, g = jnp.split(proj, [H * dk, 2 * H * dk, 2 * H * dk + H * dv], axis=-1)
    pos = jnp.arange(T)
    q = rotary(q.reshape(Bsz, T, H, dk).astype(f32), pos)
    k = rotary(k.reshape(Bsz, T, H, dk).astype(f32), pos) * (dk ** -0.5)
    v = v.reshape(Bsz, T, H, dv).astype(f32)

    log_gamma = jnp.log1p(-jnp.exp2(-5.0 - jnp.arange(H, dtype=f32)))
    idx = jnp.arange(C, dtype=f32)
    diff = idx[:, None] - idx[None, :]
    intra_decay = jnp.where(diff >= 0.0,
                            jnp.exp(log_gamma[:, None, None] * jnp.maximum(diff, 0.0)[None]),
                            0.0)
    q_decay = jnp.exp(log_gamma[:, None] * (idx + 1.0))[None, :, :, None]
    k_decay = jnp.exp(log_gamma[:, None] * (C - 1.0 - idx))[None, :, :, None]
    chunk_decay = jnp.exp(log_gamma * C)[None, :, None, None]

    def to_chunks(a):
        return a.reshape(Bsz, n_chunks, C, H, a.shape[-1]).transpose(1, 0, 3, 2, 4)

    def step(state, qkv):
        qc, kc, vc = qkv
        scores = jnp.einsum('bhid,bhjd->bhij', qc, kc) * intra_decay
        inner = jnp.einsum('bhij,bhje->bhie', scores, vc)
        cross = jnp.einsum('bhid,bhde->bhie', qc * q_decay, state)
        new_state = chunk_decay * state + jnp.einsum('bhjd,bhje->bhde', kc * k_decay, vc)
        return new_state, inner + cross

    state0 = jnp.zeros((Bsz, H, dk, dv), f32)
    _, out = lax.scan(step, state0, (to_chunks(q), to_chunks(k), to_chunks(v)))
    out = out.transpose(1, 0, 3, 2, 4).reshape(Bsz, T, H, dv)
    out = out * lax.rsqrt(jnp.mean(out * out, axis=-1, keepdims=True) + NORM_EPS)
    y = jax.nn.silu(g) * out.reshape(Bsz, T, H * dv).astype(x.dtype)
    return y @ w_out


def setup_inputs(seed: int = 0) -> dict:
    key = jax.random.key(seed)
    ks = jax.random.split(key, 16)
    f32 = jnp.float32

    def w(k, shape, fan_in):
        return jax.random.normal(k, shape, f32) * (fan_in ** -0.5)

    ret_in_width = 2 * RET_HEADS * RET_QK_DIM + 2 * RET_HEADS * RET_V_DIM
    return {
        "x": jax.random.normal(ks[0], (BATCH, SEQ, D_MODEL), f32),
        "p": jax.random.normal(ks[1], (DEPTH, BATCH, SEQ, D_PLE), f32),
        "norm_g": 1.0 + 0.05 * jax.random.normal(ks[2], (DEPTH, N_NORMS, D_MODEL), f32),
        "ffn1_w_gate": w(ks[3], (DEPTH, D_MODEL, D_FF), D_MODEL),
        "ffn1_w_up": w(ks[4], (DEPTH, D_MODEL, D_FF), D_MODEL),
        "ffn1_w_down": w(ks[5], (DEPTH, D_FF, D_MODEL), D_FF),
        "ffn2_w_gate": w(ks[6], (DEPTH, D_MODEL, D_FF), D_MODEL),
        "ffn2_w_up": w(ks[7], (DEPTH, D_MODEL, D_FF), D_MODEL),
        "ffn2_w_down": w(ks[8], (DEPTH, D_FF, D_MODEL), D_FF),
        "conv_w_in": w(ks[9], (N_CONV_LAYERS, D_MODEL, 3 * D_MODEL), D_MODEL),
        "conv_w": w(ks[10], (N_CONV_LAYERS, CONV_WIDTH, D_MODEL), CONV_WIDTH),
        "conv_w_out": w(ks[11], (N_CONV_LAYERS, D_MODEL, D_MODEL), D_MODEL),
        "ret_w_in": w(ks[12], (N_RET_LAYERS, D_MODEL, ret_in_width), D_MODEL),
        "ret_w_out": w(ks[13], (N_RET_LAYERS, RET_HEADS * RET_V_DIM, D_MODEL), RET_HEADS * RET_V_DIM),
        "ple_w_proj": w(ks[14], (DEPTH, D_PLE, D_MODEL), D_PLE),
        "ple_w_gate": w(ks[15], (DEPTH, D_MODEL, D_MODEL), D_MODEL),
    }


def reference(x, p, norm_g, ffn1_w_gate, ffn1_w_up, ffn1_w_down, ffn2_w_gate, ffn2_w_up, ffn2_w_down,
              conv_w_in, conv_w, conv_w_out, ret_w_in, ret_w_out, ple_w_proj, ple_w_gate):
    for i in range(DEPTH):
        g = norm_g[i]
        h = swiglu(rmsnorm(x, g[FFN1_PRE]), ffn1_w_gate[i], ffn1_w_up[i], ffn1_w_down[i])
        x = x + 0.5 * rmsnorm(h, g[FFN1_POST])
        xn = rmsnorm(x, g[MIX_PRE])
        j = i // N_MIXERS
        if i % N_MIXERS == 0:
            h = short_conv_mixer(xn, conv_w_in[j], conv_w[j], conv_w_out[j])
        else:
            h = retention_mixer(xn, ret_w_in[j], ret_w_out[j])
        x = x + rmsnorm(h, g[MIX_POST])
        h = swiglu(rmsnorm(x, g[FFN2_PRE]), ffn2_w_gate[i], ffn2_w_up[i], ffn2_w_down[i])
        x = x + 0.5 * rmsnorm(h, g[FFN2_POST])
        gate = jax.nn.sigmoid(rmsnorm(x, g[PLE_PRE]) @ ple_w_gate[i])
        e = (p[i].astype(x.dtype) @ ple_w_proj[i]) * gate
        x = x + rmsnorm(e, g[PLE_POST])
    return x
```

```python
import numpy as np
import concourse.bass as bass
import concourse.mybir as mybir
from concourse.bass_utils import run_bass_kernel_spmd

F32 = mybir.dt.float32
BF16 = mybir.dt.bfloat16
AF = mybir.ActivationFunctionType
ALU = mybir.AluOpType

P = 128
D = 1024
KC = D // P
DFF = 2816
FC = DFF // P
DPLE = 256
TT = 512
NCH = TT // P
EPS = 1e-6
H = 4
DK = 256
DV = 512
CH = 128
N_CORES = 8
SEQ = 4096
BLK = 256

FFN1_PRE, FFN1_POST, MIX_PRE, MIX_POST, FFN2_PRE, FFN2_POST, PLE_PRE, PLE_POST = range(8)


class View:
    __slots__ = ("ap", "blocks")

    def __init__(self, ap, blocks):
        self.ap = ap
        self.blocks = blocks


class Buf:
    def __init__(self, ap, space, off, shape, esz):
        self.ap, self.space, self.off, self.shape, self.esz = ap, space, off, tuple(shape), esz
        st, acc = [], 1
        for n in reversed(self.shape):
            st.append(acc)
            acc *= n
        self.strides = tuple(reversed(st))
        self.nbytes = acc * esz

    def __getitem__(self, idx):
        if not isinstance(idx, tuple):
            idx = (idx,)
        idx = idx + (slice(None),) * (len(self.shape) - len(idx))
        rng = []
        for i, n in zip(idx, self.shape):
            if isinstance(i, int):
                rng.append((i, i + 1))
            else:
                a, b, s = i.indices(n)
                assert s == 1
                rng.append((a, b))
        L = 1
        d = len(rng) - 1
        while d >= 0:
            a, b = rng[d]
            if a == 0 and b == self.shape[d]:
                L *= self.shape[d]
                d -= 1
                continue
            break
        starts = [0]
        if d >= 0:
            a, b = rng[d]
            L *= (b - a)
            starts = [a * self.strides[d]]
            for dd in range(d - 1, -1, -1):
                a, b = rng[dd]
                starts = [s0 + i * self.strides[dd] for i in range(a, b) for s0 in starts]
        blocks = set()
        for s0 in starts:
            lo = (self.off + s0 * self.esz) // BLK
            hi = (self.off + (s0 + L) * self.esz - 1) // BLK
            for b in range(lo, hi + 1):
                blocks.add((self.space, b))
        return View(self.ap[(slice(None),) + idx], blocks)

    def all(self):
        return self[tuple(slice(None) for _ in self.shape)]


class Op:
    __slots__ = ("eng", "fn", "deps", "sig", "sem", "val", "inc", "is_dma", "tag")

    def __init__(self, eng, fn, is_dma):
        self.eng, self.fn, self.is_dma = eng, fn, is_dma
        self.deps = set()
        self.sig = is_dma
        self.sem = None
        self.val = 0
        self.inc = 16 if is_dma else 1


class Prog:
    ENGS = ("pe", "act", "dve", "pool", "sp")
    NDMA_SEM = 12
    NSEM = {"pool": 3}

    def __init__(self):
        self.ops = {e: [] for e in self.ENGS}
        self.state = {}
        self.dma_count = {e: 0 for e in self.ENGS}
        self.dma_last = {}
        self.tag = ""

    def add(self, eng, fn, reads=(), writes=(), dma=False):
        op = Op(eng, fn, dma)
        op.tag = self.tag
        deps = op.deps
        st = self.state
        for v in reads:
            for b in v.blocks:
                e = st.get(b)
                if e is not None and e[0] is not None:
                    deps.add(e[0])
        for v in writes:
            for b in v.blocks:
                e = st.get(b)
                if e is not None:
                    if e[0] is not None:
                        deps.add(e[0])
                    deps.update(e[1])
        for v in reads:
            for b in v.blocks:
                e = st.get(b)
                if e is None:
                    st[b] = [None, [op]]
                else:
                    e[1].append(op)
        for v in writes:
            for b in v.blocks:
                st[b] = [op, []]
        deps.discard(op)
        if dma:
            n = self.dma_count[eng]
            self.dma_count[eng] = n + 1
            nsem = self.NSEM.get(eng, self.NDMA_SEM)
            slot = n % nsem
            prev = self.dma_last.get((eng, slot))
            if prev is not None:
                deps.add(prev)
            self.dma_last[(eng, slot)] = op
            op.sem = (eng, slot)
            op.val = 16 * (n // nsem + 1)
        self.ops[eng].append(op)
        return op

    def finalize(self):
        for e in self.ENGS:
            for op in self.ops[e]:
                for d in op.deps:
                    if d.is_dma:
                        continue
                    if d.eng == "pe" and e == "pe":
                        continue
                    d.sig = True
        for e in self.ENGS:
            cnt = 0
            for op in self.ops[e]:
                if op.is_dma:
                    continue
                if op.sig:
                    cnt += 1
                    op.sem = (e, "c")
                    op.val = cnt

    def emit_engine(self, ename, eng, sems):
        seen = {}
        for op in self.ops[ename]:
            need = {}
            for d in op.deps:
                if (not d.is_dma) and d.eng == "pe" and ename == "pe":
                    continue
                if d.val > need.get(d.sem, 0):
                    need[d.sem] = d.val
            for k, v in need.items():
                if seen.get(k, 0) >= v:
                    continue
                eng.wait_ge(sems[k], v)
                seen[k] = v
            if op.fn is not None:
                ins = op.fn(eng)
                if op.sig:
                    ins.then_inc(sems[op.sem], op.inc)


class Ring:
    def __init__(self, bufs):
        self.bufs = bufs
        self.i = 0

    def next(self):
        b = self.bufs[self.i % len(self.bufs)]
        self.i += 1
        return b


def build(T=SEQ, layers=(0, 1)):
    nc = bass.Bass("TRN2", target_bir_lowering=False)
    ntile = T // TT
    pg = Prog()

    def din(name, shape):
        return nc.dram_tensor(name, list(shape), F32, kind="ExternalInput").ap()

    x_d = din("x", [T, D])
    p_d = din("p", [2, T, DPLE])
    ng_d = din("norm_g", [2, 8, D])
    w_f1g = din("ffn1_w_gate", [2, D, DFF])
    w_f1u = din("ffn1_w_up", [2, D, DFF])
    w_f1d = din("ffn1_w_down", [2, DFF, D])
    w_f2g = din("ffn2_w_gate", [2, D, DFF])
    w_f2u = din("ffn2_w_up", [2, D, DFF])
    w_f2d = din("ffn2_w_down", [2, DFF, D])
    w_cin = din("conv_w_in", [1, D, 3 * D])
    w_cw = din("conv_w", [1, 3, D])
    w_cout = din("conv_w_out", [1, D, D])
    w_rin = din("ret_w_in", [1, D, 2 * H * DK + 2 * H * DV])
    w_rout = din("ret_w_out", [1, H * DV, D])
    w_pp = din("ple_w_proj", [2, DPLE, D])
    w_pg = din("ple_w_gate", [2, D, D])
    c_ident = din("c_ident", [P, P])
    c_mask = din("c_mask", [P, H, P])
    c_dq = din("c_dq", [P, H, P])
    c_dk = din("c_dk", [P, H])
    c_cos = din("c_cos", [P, T])
    c_sin = din("c_sin", [P, T])
    y_d = nc.dram_tensor("y", [T, D], F32, kind="ExternalOutput").ap()
    WSCR_ELEMS = 2 * (2 * 3 * D * DFF) + D * 3 * D + D * D + D * (2 * H * DK + 2 * H * DV) + H * DV * D + 2 * (DPLE * D + D * D)
    wscr = nc.dram_tensor("wscr", [WSCR_ELEMS], BF16, kind="Internal").ap()
    wreg = {}
    wscr_cur = [0]
    cur_tile = [0]

    ARENA_BYTES = 206 * 1024
    arena_cm = nc.sbuf_tensor("arena", [P, ARENA_BYTES // 4], F32)
    psum_cm = nc.psum_tensor("ps", [P, 8, 512], F32)
    arena = arena_cm.__enter__()
    psum = psum_cm.__enter__()

    cursor = [0]

    def carve_at(off, shape, dt):
        esz = 4 if dt == F32 else 2
        n = int(np.prod(shape))
        nbytes = n * esz
        assert off % 4 == 0
        ap = arena[:, off // 4:(off + nbytes + 3) // 4]
        if dt == BF16:
            ap = ap.bitcast(BF16)
        if len(shape) == 2:
            ap = ap.rearrange("p (a b) -> p a b", a=shape[0])
        elif len(shape) == 3:
            ap = ap.rearrange("p (a b c) -> p a b c", a=shape[0], b=shape[1])
        return Buf(ap, "sb", off, shape, esz)

    def carve(shape, dt):
        esz = 4 if dt == F32 else 2
        nbytes = int(np.prod(shape)) * esz
        off = cursor[0]
        cursor[0] = (off + nbytes + BLK - 1) // BLK * BLK
        assert cursor[0] <= ARENA_BYTES, ("arena overflow", cursor[0])
        return carve_at(off, shape, dt)

    X = carve([KC, TT], F32)
    S = carve([H * 2, DV], F32)
    Sb = carve([H * 2, DV], BF16)
    HALO = carve([KC, 2], F32)
    IDF = carve([P], F32)
    IDB = carve([P], BF16)
    ONES_D = carve([P], BF16)
    ONES_V = carve([P], BF16)
    G = carve([P], F32)
    GH = carve([P], F32)
    CW = carve([P], F32)
    MASK = carve([H, P], F32)
    DQ = carve([H, P], F32)
    DKH = carve([H], F32)
    EPSC = carve([1], F32)
    PT = carve([2, 2, TT], BF16)
    CS = carve([2, TT], F32)
    XS = Ring([carve([D], F32) for _ in range(2)])
    PS_ST = Ring([carve([DPLE], F32) for _ in range(2)])
    LOADT = carve([P], F32)
    XN = carve([KC, TT], BF16)
    RSTD = Ring([carve([TT], F32) for _ in range(2)])
    SQ = Ring([carve([TT], BF16) for _ in range(6)])
    TMPA = Ring([carve([TT], F32) for _ in range(3)])
    TMPB = Ring([carve([TT], F32) for _ in range(3)])
    ST_R = Ring([carve([P], BF16) for _ in range(5)])
    KT_R = Ring([carve([2 * P], BF16) for _ in range(5)])
    RSG_R = Ring([carve([P], F32) for _ in range(3)])
    WBYTES = 40 * 1024
    w_base = cursor[0]
    cursor[0] += WBYTES
    scr0 = cursor[0]
    Hh = carve_at(scr0, [FC, TT], BF16)
    Y = carve_at(scr0 + 22 * 1024, [KC, TT], F32)
    U = carve_at(scr0, [KC, TT + 2], F32)
    BV = carve_at(scr0 + 38 * 1024, [KC, TT], BF16)
    YR = carve_at(scr0, [H * 4, TT], BF16)
    Q = carve_at(scr0 + 16 * 1024, [H * 2, TT], BF16)
    QD = carve_at(scr0 + 24 * 1024, [H * 2, TT], BF16)
    Kb = carve_at(scr0 + 32 * 1024, [H * 2, TT], BF16)
    V = carve_at(scr0 + 40 * 1024, [NCH, H * DV], BF16)
    assert scr0 + 56 * 1024 <= ARENA_BYTES, scr0
    OSLOT = [carve_at(scr0 + i * 4096, [D], F32) for i in range(NCH)]
    ISLOT = [carve_at(scr0 + 16 * 1024 + i * 4096, [D], F32) for i in range(NCH)]
    print("sbuf bytes used", scr0 + 56 * 1024)

    PSB = Buf(psum, "ps", 0, [8, 512], 4)
    PSB16 = Buf(psum.bitcast(BF16), "ps", 0, [8, 1024], 2)
    ps_i = [0]

    def ps_next():
        b = ps_i[0] % 6
        ps_i[0] += 1
        return b

    ss_i = [0]

    def ss_next():
        b = 6 + ss_i[0] % 2
        ss_i[0] += 1
        return b

    w_cur = [0]

    def walloc(shape):
        nbytes = int(np.prod(shape)) * 2
        nb = (nbytes + BLK - 1) // BLK * BLK
        assert nb <= WBYTES
        if w_cur[0] + nb > WBYTES:
            w_cur[0] = 0
        off = w_base + w_cur[0]
        w_cur[0] += nb
        return carve_at(off, shape, BF16)

    def mm(out, lhsT, rhs, start, stop):
        return pg.add("pe", lambda t: t.matmul(out.ap, lhsT.ap, rhs.ap, start=start, stop=stop),
                      reads=[lhsT, rhs], writes=[out])

    def warm(n):
        for _ in range(n):
            pg.add("pe", lambda t: t.matmul(PSB[5].ap, ONES_D.all().ap, XN[0].ap, start=True, stop=True),
                   reads=[ONES_D.all()], writes=[])

    def tr(out, in_, ident):
        return pg.add("pe", lambda t: t.transpose(out.ap, in_.ap, ident.ap), reads=[in_, ident], writes=[out])

    def act(out, in_, func, scale=1.0, bias=None, extra_reads=()):
        rd = [in_] + list(extra_reads)
        if bias is not None:
            rd.append(bias)
        sc = scale.ap if isinstance(scale, View) else scale
        if isinstance(scale, View):
            rd.append(scale)
        if bias is not None:
            f = lambda a: a.activation(out.ap, in_.ap, func, bias=bias.ap, scale=sc)
        else:
            f = lambda a: a.activation(out.ap, in_.ap, func, scale=sc)
        return pg.add("act", f, reads=rd, writes=[out])

    def pl(e):
        if e == "pool" and cur_tile[0] == 0:
            return "dve"
        return e

    def tt(out, in0, in1, op, in1_ap=None, eng="dve"):
        a1 = in1.ap if in1_ap is None else in1_ap
        return pg.add(pl(eng), lambda v: v.tensor_tensor(out=out.ap, in0=in0.ap, in1=a1, op=op),
                      reads=[in0, in1], writes=[out])

    def stt(out, in0, scalar, in1, op0, op1, eng="dve"):
        if isinstance(scalar, View):
            return pg.add(pl(eng), lambda v: v.scalar_tensor_tensor(out=out.ap, in0=in0.ap, scalar=scalar.ap, in1=in1.ap,
                                                                    op0=op0, op1=op1),
                          reads=[in0, scalar, in1], writes=[out])
        return pg.add(pl(eng), lambda v: v.scalar_tensor_tensor(out=out.ap, in0=in0.ap, scalar=scalar, in1=in1.ap,
                                                                op0=op0, op1=op1),
                      reads=[in0, in1], writes=[out])

    def ts(out, in0, scalar, op, eng="dve"):
        if isinstance(scalar, View):
            return pg.add(pl(eng), lambda v: v.tensor_scalar(out=out.ap, in0=in0.ap, scalar1=scalar.ap, scalar2=None, op0=op),
                          reads=[in0, scalar], writes=[out])
        return pg.add(pl(eng), lambda v: v.tensor_scalar(out=out.ap, in0=in0.ap, scalar1=scalar, scalar2=None, op0=op),
                      reads=[in0], writes=[out])

    def vcopy(out, in_):
        return pg.add("dve", lambda v: v.tensor_copy(out=out.ap, in_=in_.ap), reads=[in_], writes=[out])

    def vmemset(out, val):
        return pg.add("dve", lambda v: v.memset(out.ap, val), writes=[out])

    def vrecip(out, in_):
        return pg.add("dve", lambda v: v.reciprocal(out=out.ap, in_=in_.ap), reads=[in_], writes=[out])

    def dma_in(eng, out, src_ap):
        if eng == "pool":
            return pg.add("pool", lambda g: g.dma_start(out=out.ap, in_=src_ap), writes=[out], dma=True)
        return pg.add("sp", lambda s: s.dma_start(out=out.ap, in_=src_ap), writes=[out], dma=True)

    def dma_out(dst_ap, src):
        return pg.add("act", lambda s: s.dma_start(out=dst_ap, in_=src.ap), reads=[src], dma=True)

    def wload(src_ap, shape, key):
        n = P * int(np.prod(shape))
        if key not in wreg:
            off = wscr_cur[0]
            wscr_cur[0] += n
            assert wscr_cur[0] <= WSCR_ELEMS
            if len(shape) == 2:
                dst = wscr[off:off + n].rearrange("(p k c) -> p k c", p=P, k=shape[0])
            else:
                dst = wscr[off:off + n].rearrange("(p c) -> p c", p=P)
            cops = []
            nk = shape[0]
            step = 8 if nk > 8 else nk
            for k0 in range(0, nk, step):
                k1 = min(nk, k0 + step)
                d_ = dst[:, k0:k1]
                s_ = src_ap[:, k0:k1]
                cops.append(pg.add("pool", lambda g, d_=d_, s_=s_: g.dma_start(out=d_, in_=s_), dma=True))
            wreg[key] = (off, cops, dst)
        off, cops, dst = wreg[key]
        wb = walloc(shape)
        op = dma_in("sp", wb.all(), dst)
        op.deps.update(cops)
        return wb

    def wview(w, l, c0, c1):
        return w[l].rearrange("(k p) n -> p k n", p=P)[:, :, c0:c1]

    vmemset(S.all(), 0.0)
    vmemset(Sb.all(), 0.0)
    vmemset(HALO.all(), 0.0)
    vmemset(EPSC.all(), EPS)
    vmemset(LOADT.all(), 0.0)
    dma_in("sp", IDF.all(), c_ident)
    dma_in("pool", IDB.all(), c_ident)
    dma_in("sp", MASK.all(), c_mask)
    dma_in("sp", DQ.all(), c_dq)
    dma_in("sp", DKH.all(), c_dk)
    t_ones = TMPA.next()
    vmemset(t_ones[0:P], 1.0 / D)
    vcopy(ONES_D.all(), t_ones[0:P])
    t_ones = TMPA.next()
    vmemset(t_ones[0:P], 1.0 / DV)
    vcopy(ONES_V.all(), t_ones[0:P])
    xs0 = XS.next()
    dma_in("sp", xs0[0:P], ng_d.rearrange("l n (c p) -> (l n c) p", p=P))
    b = ps_next()
    tr(PSB[b, 0:P], xs0[0:P], IDF.all())
    vcopy(G.all(), PSB[b, 0:P])
    ts(GH.all(), G.all(), 0.5, ALU.mult)
    dma_in("sp", Buf(LOADT.ap[0:24], "sb", LOADT.off, [P], 4).all(), w_cw[0].rearrange("k (c p) -> (k c) p", p=P))
    b = ps_next()
    tr(PSB[b, 0:P], LOADT.all(), IDF.all())
    vcopy(CW.all(), PSB[b, 0:P])

    def gcol(Gb, l, n, c):
        j = (l * 8 + n) * 8 + c
        return Gb[j:j + 1]

    def rstd_from(ssb):
        r = RSTD.next()
        act(r.all(), PSB[ssb], AF.Sqrt, bias=EPSC.all())
        vrecip(r.all(), r.all())
        return r

    def prenorm(l, n):
        pg.tag = pg.tag.split('.')[0] + '.pre'
        ssb = ss_next()
        for c in range(KC):
            sq = SQ.next()
            act(sq.all(), X[c], AF.Square)
            mm(PSB[ssb], ONES_D.all(), sq.all(), c == 0, c == KC - 1)
        r = rstd_from(ssb)
        for c in range(KC):
            if c >= 5 and cur_tile[0] > 0:
                tmp = TMPB.next()
                tt(tmp.all(), X[c], r.all(), ALU.mult, eng="pool")
                act(XN[c], tmp.all(), AF.Copy, scale=gcol(G, l, n, c))
            else:
                stt(XN[c], X[c], gcol(G, l, n, c), r.all(), ALU.mult, ALU.mult)

    class PostNorm:
        def __init__(self, l, n, half):
            self.ssb = ss_next()
            self.n = 0
            self.pending = None
            self.l, self.nn = l, n
            self.Gb = GH if half else G
            self.scaled = True

        def _flush(self):
            if self.pending is not None:
                sq = self.pending
                self.pending = None
                mm(PSB[self.ssb], ONES_D.all(), sq.all(), self.n == 0, self.n == KC - 1)
                self.n += 1

        def add_from_psum(self, j, psv):
            self._flush()
            act(Y[j], psv, AF.Copy, scale=gcol(self.Gb, self.l, self.nn, j))
            sq = SQ.next()
            act(sq.all(), psv, AF.Square)
            self.pending = sq

        def add_from_y(self, j):
            self.scaled = False
            self._flush()
            sq = SQ.next()
            act(sq.all(), Y[j], AF.Square)
            self.pending = sq

        def finish(self):
            self._flush()
            pg.tag = pg.tag.split('.')[0] + '.post'
            assert self.n == KC
            r = rstd_from(self.ssb)
            for j in range(KC):
                t = TMPA.next()
                if self.scaled:
                    e = "pool" if j in (1, 4, 7) else "dve"
                    tt(t.all(), Y[j], r.all(), ALU.mult, eng=e)
                    tt(X[j], t.all(), X[j], ALU.add, eng=e)
                else:
                    tt(t.all(), Y[j], r.all(), ALU.mult, eng="pool")
                    stt(X[j], t.all(), gcol(self.Gb, self.l, self.nn, j), X[j], ALU.mult, ALU.add)

    def ffn(l, wg, wu, wd, n_pre, n_post, tag):
        pg.tag = tag
        prenorm(l, n_pre)
        pg.tag = tag + '.gu'
        for f2 in range(FC // 2):
            g_t = wload(wview(wg, l, f2 * 256, (f2 + 1) * 256), [KC, 256], (tag, 'g', l, f2))
            u_t = wload(wview(wu, l, f2 * 256, (f2 + 1) * 256), [KC, 256], (tag, 'u', l, f2))
            for hf in range(2):
                f = 2 * f2 + hf
                bg, bu = ps_next(), ps_next()
                for k in range(KC):
                    mm(PSB[bg], g_t[k, hf * P:(hf + 1) * P], XN[k], k == 0, k == KC - 1)
                for k in range(KC):
                    mm(PSB[bu], u_t[k, hf * P:(hf + 1) * P], XN[k], k == 0, k == KC - 1)
                sg = TMPB.next()
                act(sg.all(), PSB[bg], AF.Silu)
                tt(Hh[f], sg.all(), PSB[bu], ALU.mult)
        pg.tag = tag + '.dn'
        pn = PostNorm(l, n_post, True)
        for j2 in range(KC // 2):
            d_t = wload(wd[l].rearrange("(k p) n -> p k n", p=P)[:, :, j2 * 256:(j2 + 1) * 256], [FC, 256], (tag, 'd', l, j2))
            for hf in range(2):
                j = 2 * j2 + hf
                by = ps_next()
                for f in range(FC):
                    mm(PSB[by], d_t[f, hf * P:(hf + 1) * P], Hh[f], f == 0, f == FC - 1)
                pn.add_from_psum(j, PSB[by])
        pn.finish()

    def conv_mixer(l):
        pg.tag = 'conv'
        prenorm(l, MIX_PRE)
        pg.tag = 'conv.in'
        for m2 in range(KC // 2):
            wb_ = wload(wview(w_cin, 0, m2 * 256, (m2 + 1) * 256), [KC, 256], ('cb', m2))
            wc_ = wload(wview(w_cin, 0, D + m2 * 256, D + (m2 + 1) * 256), [KC, 256], ('cc', m2))
            wh_ = wload(wview(w_cin, 0, 2 * D + m2 * 256, 2 * D + (m2 + 1) * 256), [KC, 256], ('ch', m2))
            for hf in range(2):
                m = 2 * m2 + hf
                bb, bc, bh = ps_next(), ps_next(), ps_next()
                for k in range(KC):
                    mm(PSB[bc], wc_[k, hf * P:(hf + 1) * P], XN[k], k == 0, k == KC - 1)
                for k in range(KC):
                    mm(PSB[bh], wh_[k, hf * P:(hf + 1) * P], XN[k], k == 0, k == KC - 1)
                for k in range(KC):
                    mm(PSB[bb], wb_[k, hf * P:(hf + 1) * P], XN[k], k == 0, k == KC - 1)
                cs = TMPB.next()
                act(cs.all(), PSB[bc], AF.Copy)
                bsb = TMPB.next()
                act(bsb.all(), PSB[bb], AF.Copy)
                vcopy(U[m, 0:2], HALO[m])
                tt(U[m, 2:TT + 2], cs.all(), PSB[bh], ALU.mult)
                v = TMPA.next()
                act(v.all(), U[m, 2:TT + 2], AF.Copy, scale=CW[16 + m:17 + m])
                stt(v.all(), U[m, 1:TT + 1], CW[8 + m:9 + m], v.all(), ALU.mult, ALU.add)
                stt(v.all(), U[m, 0:TT], CW[m:m + 1], v.all(), ALU.mult, ALU.add)
                tt(BV[m], v.all(), bsb.all(), ALU.mult, eng="pool")
                vcopy(HALO[m], U[m, TT:TT + 2])
        pg.tag = 'conv.out'
        pn = PostNorm(l, MIX_POST, False)
        for j2 in range(KC // 2):
            o_t = wload(wview(w_cout, 0, j2 * 256, (j2 + 1) * 256), [KC, 256], ('co', j2))
            for hf in range(2):
                j = 2 * j2 + hf
                by = ps_next()
                for m in range(KC):
                    mm(PSB[by], o_t[m, hf * P:(hf + 1) * P], BV[m], m == 0, m == KC - 1)
                pn.add_from_psum(j, PSB[by])
        pn.finish()

    GAMMA = [1.0 - 2.0 ** (-5 - h) for h in range(H)]
    GAMMA_C = [float(np.float32(g) ** np.float32(CH)) for g in GAMMA]

    def retention(l):
        pg.tag = 'ret'
        prenorm(l, MIX_PRE)
        pg.tag = 'ret.qk'
        qoff, koff, voff, goff = 0, H * DK, 2 * H * DK, 2 * H * DK + H * DV
        for h in range(H):
            for which in range(2):
                base = (qoff if which == 0 else koff) + h * DK
                w_t = wload(wview(w_rin, 0, base, base + DK), [KC, DK], ('rqk', h, which))
                b1, b2 = ps_next(), ps_next()
                for k in range(KC):
                    mm(PSB[b1], w_t[k, 0:P], XN[k], k == 0, k == KC - 1)
                for k in range(KC):
                    mm(PSB[b2], w_t[k, P:2 * P], XN[k], k == 0, k == KC - 1)
                dst = Q if which == 0 else Kb
                a = TMPA.next()
                bt = TMPB.next()
                tt(a.all(), PSB[b1], CS[0], ALU.mult)
                tt(bt.all(), PSB[b2], CS[1], ALU.mult)
                tt(dst[2 * h], a.all(), bt.all(), ALU.subtract, eng="pool")
                a = TMPA.next()
                bt = TMPB.next()
                tt(a.all(), PSB[b2], CS[0], ALU.mult)
                tt(bt.all(), PSB[b1], CS[1], ALU.mult)
                tt(dst[2 * h + 1], a.all(), bt.all(), ALU.add, eng="pool")
                if which == 0:
                    dq_b = DQ[h].ap.unsqueeze(1).to_broadcast([P, NCH, P])
                    for hh in range(2):
                        qd3 = QD[2 * h + hh].ap.rearrange("p (a b) -> p a b", a=NCH)
                        q3 = Q[2 * h + hh].ap.rearrange("p (a b) -> p a b", a=NCH)
                        pg.add(pl("pool"), lambda v, qd3=qd3, q3=q3, dq_b=dq_b: v.tensor_tensor(out=qd3, in0=q3, in1=dq_b, op=ALU.mult),
                               reads=[Q[2 * h + hh], DQ[h]], writes=[QD[2 * h + hh]])
        pg.tag = 'ret.v'
        for vb in range(H):
            w_t = wload(wview(w_rin, 0, voff + vb * DV, voff + (vb + 1) * DV), [KC, DV], ('rv', vb))
            for n in range(NCH):
                bv_ = ps_next()
                for k in range(KC):
                    mm(PSB[bv_], XN[k, n * P:(n + 1) * P], w_t[k], k == 0, k == KC - 1)
                act(V[n, vb * DV:(vb + 1) * DV], PSB[bv_], AF.Copy)
        pg.tag = 'ret.core'
        for n in range(NCH):
            tsl = slice(n * P, (n + 1) * P)
            sts, kts, bos, sqs = [], [], [], []
            for h in range(H):
                bs = ps_next()
                for dc in range(2):
                    mm(PSB[bs, 0:P], Kb[2 * h + dc, tsl], Q[2 * h + dc, tsl], dc == 0, dc == 1)
                for dc in range(2):
                    tr(PSB16[bs, 512 + dc * P:512 + (dc + 1) * P], Kb[2 * h + dc, tsl], IDB.all())
                st_ = ST_R.next()
                s_ap = PSB[bs, 0:P].ap
                pg.add("dve", lambda v, o=st_.all().ap, i0=s_ap, i1=MASK[h].ap: v.tensor_tensor(out=o, in0=i0, in1=i1, op=ALU.mult),
                       reads=[PSB[bs], MASK[h]], writes=[st_.all()])
                kt = KT_R.next()
                k_ap = PSB16[bs, 512:512 + 2 * P].ap
                pg.add("dve", lambda v, o=kt.all().ap, i0=k_ap, sc=DKH[h:h + 1].ap: v.tensor_scalar(out=o, in0=i0, scalar1=sc, scalar2=None, op0=ALU.mult),
                       reads=[PSB[bs], DKH[h:h + 1]], writes=[kt.all()])
                sts.append(st_)
                kts.append(kt)
            for h in range(H):
                bo = ps_next()
                for ec in range(4):
                    osl = slice(ec * P, (ec + 1) * P)
                    mm(PSB[bo, osl], V[n, h * DV + ec * P:h * DV + (ec + 1) * P], sts[h].all(), True, False)
                    for dc in range(2):
                        mm(PSB[bo, osl], Sb[2 * h + dc, ec * P:(ec + 1) * P], QD[2 * h + dc, tsl], False, dc == 1)
                sq = SQ.next()
                act(sq.all(), PSB[bo], AF.Square)
                bos.append(bo)
                sqs.append(sq)
            for h in range(H):
                bo, sq = bos[h], sqs[h]
                bgn = ss_next()
                for ec in range(4):
                    mm(PSB[bgn, 0:P], ONES_V.all(), sq[ec * P:(ec + 1) * P], ec == 0, ec == 3)
                rs = RSG_R.next()
                act(rs.all(), PSB[bgn, 0:P], AF.Sqrt, bias=EPSC.all())
                vrecip(rs.all(), rs.all())
                rs_b = rs.all().ap.unsqueeze(1).to_broadcast([P, 4, P])
                o3 = PSB[bo].ap.rearrange("p (a b) -> p a b", a=4)
                yr3 = YR[4 * h:4 * h + 4, tsl]
                pg.add("dve", lambda v, yr3=yr3, o3=o3, rs_b=rs_b: v.tensor_tensor(out=yr3.ap, in0=o3, in1=rs_b, op=ALU.mult),
                       reads=[PSB[bo], rs.all()], writes=[yr3])
            for h in range(H):
                for dc in range(2):
                    bd = ps_next()
                    mm(PSB[bd], kts[h][dc * P:(dc + 1) * P], V[n, h * DV:(h + 1) * DV], True, True)
                    stt(S[2 * h + dc], S[2 * h + dc], GAMMA_C[h], PSB[bd], ALU.mult, ALU.add)
                    act(Sb[2 * h + dc], S[2 * h + dc], AF.Copy)
        pg.tag = 'ret.g'
        for gb in range(H * DV // 256):
            w_t = wload(wview(w_rin, 0, goff + gb * 256, goff + (gb + 1) * 256), [KC, 256], ('rg', gb))
            for hf in range(2):
                c = 2 * gb + hf
                bg = ps_next()
                for k in range(KC):
                    mm(PSB[bg], w_t[k, hf * P:(hf + 1) * P], XN[k], k == 0, k == KC - 1)
                sgt = TMPB.next()
                act(sgt.all(), PSB[bg], AF.Silu)
                tt(YR[c], YR[c], sgt.all(), ALU.mult)
        pg.tag = 'ret.out'
        pn = PostNorm(l, MIX_POST, False)
        for j2 in range(KC // 2):
            o_t = wload(wview(w_rout, 0, j2 * 256, (j2 + 1) * 256), [H * 4, 256], ('ro', j2))
            for hf in range(2):
                j = 2 * j2 + hf
                by = ps_next()
                for c in range(H * 4):
                    mm(PSB[by], o_t[c, hf * P:(hf + 1) * P], YR[c], c == 0, c == H * 4 - 1)
                pn.add_from_psum(j, PSB[by])
        pn.finish()

    def ple(l):
        pg.tag = 'ple'
        prenorm(l, PLE_PRE)
        pg.tag = 'ple.main'
        pn = PostNorm(l, PLE_POST, False)
        for j2 in range(KC // 2):
            g_t = wload(wview(w_pg, l, j2 * 256, (j2 + 1) * 256), [KC, 256], ('pg', l, j2))
            p_t = wload(wview(w_pp, l, j2 * 256, (j2 + 1) * 256), [2, 256], ('pp', l, j2))
            for hf in range(2):
                j = 2 * j2 + hf
                bg, be = ps_next(), ps_next()
                for k in range(KC):
                    mm(PSB[bg], g_t[k, hf * P:(hf + 1) * P], XN[k], k == 0, k == KC - 1)
                for k in range(2):
                    mm(PSB[be], p_t[k, hf * P:(hf + 1) * P], PT[l, k], k == 0, k == 1)
                sg = TMPB.next()
                act(sg.all(), PSB[bg], AF.Sigmoid)
                tt(Y[j], sg.all(), PSB[be], ALU.mult)
                pn.add_from_y(j)
        pn.finish()

    out_dmas = []

    def load_tile(ti):
        pg.tag = 'load'
        t0 = ti * TT
        for n in range(NCH):
            dma_in("sp", ISLOT[n].all(), x_d[t0 + n * P:t0 + (n + 1) * P, :])
        for n in range(NCH):
            xs = ISLOT[n]
            for hb in range(2):
                b = ps_next()
                for q in range(4):
                    c = hb * 4 + q
                    tr(PSB[b, q * P:(q + 1) * P], xs[c * P:(c + 1) * P], IDF.all())
                dst = X[hb * 4:hb * 4 + 4, n * P:(n + 1) * P]
                src = PSB[b].ap.rearrange("p (a b) -> p a b", a=4)
                pg.add("act", lambda a, dst=dst, src=src: a.activation(dst.ap, src, AF.Copy),
                       reads=[PSB[b]], writes=[dst])
            for l in layers:
                pst = PS_ST.next()
                dma_in("sp", pst.all(), p_d[l, t0 + n * P:t0 + (n + 1) * P, :])
                b = ps_next()
                for k in range(2):
                    tr(PSB[b, k * P:(k + 1) * P], pst[k * P:(k + 1) * P], IDF.all())
                dst = PT[l, 0:2, n * P:(n + 1) * P]
                src = PSB[b, 0:2 * P].ap.rearrange("p (a b) -> p a b", a=2)
                pg.add("dve", lambda v, dst=dst, src=src: v.tensor_copy(out=dst.ap, in_=src),
                       reads=[PSB[b, 0:2 * P]], writes=[dst])
        if 1 in layers:
            dma_in("sp", CS[0], c_cos[:, t0:t0 + TT])
            dma_in("sp", CS[1], c_sin[:, t0:t0 + TT])

    def store_tile(ti):
        pg.tag = 'store'
        t0 = ti * TT
        for n in range(NCH):
            xs = OSLOT[n]
            for hb in range(2):
                b = ps_next()
                for q in range(4):
                    c = hb * 4 + q
                    tr(PSB[b, q * P:(q + 1) * P], X[c, n * P:(n + 1) * P], IDF.all())
                act(xs[hb * 4 * P:(hb * 4 + 4) * P], PSB[b], AF.Copy)
            out_dmas.append(dma_out(y_d[t0 + n * P:t0 + (n + 1) * P, :], xs.all()))

    for ti in range(ntile):
        cur_tile[0] = ti
        load_tile(ti)
        for l in layers:
            ffn(l, w_f1g, w_f1u, w_f1d, FFN1_PRE, FFN1_POST, 'f1')
            if l % 2 == 0:
                conv_mixer(l)
            else:
                retention(l)
            ffn(l, w_f2g, w_f2u, w_f2d, FFN2_PRE, FFN2_POST, 'f2')
            ple(l)
        store_tile(ti)
    fin = pg.add("sp", None)
    fin.deps.update(out_dmas)

    pg.finalize()

    sem_cms = []
    sems = {}

    def mksem(key, name):
        cm = nc.semaphore(name)
        sem_cms.append(cm)
        sems[key] = cm.__enter__()

    for e in ("pe", "act", "dve", "pool", "sp"):
        mksem((e, "c"), "c_" + e)
    for e in ("pool", "sp", "act"):
        for s_ in range(Prog.NDMA_SEM):
            mksem((e, s_), "d_%s_%d" % (e, s_))

    with nc.Block() as block:
        @block.tensor
        def _(t):
            pg.emit_engine("pe", t, sems)

        @block.scalar
        def _(a):
            pg.emit_engine("act", a, sems)

        @block.vector
        def _(v):
            pg.emit_engine("dve", v, sems)

        @block.gpsimd
        def _(g):
            pg.emit_engine("pool", g, sems)

        @block.sync
        def _(s):
            pg.emit_engine("sp", s, sems)

    for cm in reversed(sem_cms):
        cm.__exit__(None, None, None)
    psum_cm.__exit__(None, None, None)
    arena_cm.__exit__(None, None, None)
    nstat = {e: len(pg.ops[e]) for e in Prog.ENGS}
    nstat["_pe_tags"] = [op.tag for op in pg.ops["pe"]]
    return nc, nstat


def make_consts(T):
    f32 = np.float32
    ident = np.eye(P, dtype=f32)
    gam = np.array([1.0 - 2.0 ** (-5 - h) for h in range(H)], dtype=np.float64)
    idx = np.arange(CH, dtype=np.float64)
    diff = idx[None, :] - idx[:, None]
    mask = np.zeros((P, H, P), dtype=f32)
    for h in range(H):
        m = np.where(diff >= 0, gam[h] ** np.maximum(diff, 0.0), 0.0) * (DK ** -0.5)
        mask[:, h, :] = m.astype(f32)
    dq = np.zeros((P, H, P), dtype=f32)
    for h in range(H):
        row = gam[h] ** (idx + 1.0)
        dq[:, h, :] = row.astype(f32)[None, :]
    dk = np.zeros((P, H), dtype=f32)
    for h in range(H):
        dk[:, h] = (gam[h] ** (CH - 1.0 - idx) * (DK ** -0.5)).astype(f32)
    half = DK // 2
    inv_freq = (1.0 / (np.float32(10000.0) ** np.linspace(0.0, 1.0, half, dtype=f32))).astype(f32)
    pos = np.arange(T, dtype=f32)
    ang = (inv_freq[:, None] * pos[None, :]).astype(f32)
    return {
        "c_ident": ident, "c_mask": mask, "c_dq": dq, "c_dk": dk,
        "c_cos": np.cos(ang).astype(f32), "c_sin": np.sin(ang).astype(f32),
    }


_WNAMES = ["norm_g", "ffn1_w_gate", "ffn1_w_up", "ffn1_w_down", "ffn2_w_gate", "ffn2_w_up", "ffn2_w_down",
           "conv_w_in", "conv_w", "conv_w_out", "ret_w_in", "ret_w_out", "ple_w_proj", "ple_w_gate"]


def run(inputs, T, n_cores, layers=(0, 1), trace=False):
    nc, _ = build(T, layers)
    consts = make_consts(T)
    shared = {k: np.ascontiguousarray(np.asarray(inputs[k], dtype=np.float32)) for k in _WNAMES}
    shared.update(consts)
    x = np.asarray(inputs["x"], dtype=np.float32)
    p = np.asarray(inputs["p"], dtype=np.float32)
    in_maps = []
    for c in range(n_cores):
        m = dict(shared)
        m["x"] = np.ascontiguousarray(x[c])
        m["p"] = np.ascontiguousarray(p[:, c])
        in_maps.append(m)
    res = run_bass_kernel_spmd(nc, in_maps, core_ids=list(range(n_cores)), trace=trace)
    out = np.stack([np.asarray(r["y"]) for r in res.results], axis=0)
    return out, res


def kernel(**inputs):
    out, _ = run(inputs, SEQ, N_CORES)
    return out.astype(np.float32)
```

```python
import numpy as np
import concourse.bass as bass
import concourse.mybir as mybir
from concourse.bass_utils import run_bass_kernel_spmd

F32 = mybir.dt.float32
BF16 = mybir.dt.bfloat16
AF = mybir.ActivationFunctionType
ALU = mybir.AluOpType

P = 128
D = 1024
KC = D // P
DFF = 2816
FC = DFF // P
DPLE = 256
TT = 512
NCH = TT // P
EPS = 1e-6
H = 4
DK = 256
DV = 512
CH = 128
N_CORES = 8
SEQ = 4096
BLK = 256

FFN1_PRE, FFN1_POST, MIX_PRE, MIX_POST, FFN2_PRE, FFN2_POST, PLE_PRE, PLE_POST = range(8)


class View:
    __slots__ = ("ap", "blocks")

    def __init__(self, ap, blocks):
        self.ap = ap
        self.blocks = blocks


class Buf:
    def __init__(self, ap, space, off, shape, esz):
        self.ap, self.space, self.off, self.shape, self.esz = ap, space, off, tuple(shape), esz
        st, acc = [], 1
        for n in reversed(self.shape):
            st.append(acc)
            acc *= n
        self.strides = tuple(reversed(st))
        self.nbytes = acc * esz

    def __getitem__(self, idx):
        if not isinstance(idx, tuple):
            idx = (idx,)
        idx = idx + (slice(None),) * (len(self.shape) - len(idx))
        rng = []
        for i, n in zip(idx, self.shape):
            if isinstance(i, int):
                rng.append((i, i + 1))
            else:
                a, b, s = i.indices(n)
                assert s == 1
                rng.append((a, b))
        L = 1
        d = len(rng) - 1
        while d >= 0:
            a, b = rng[d]
            if a == 0 and b == self.shape[d]:
                L *= self.shape[d]
                d -= 1
                continue
            break
        starts = [0]
        if d >= 0:
            a, b = rng[d]
            L *= (b - a)
            starts = [a * self.strides[d]]
            for dd in range(d - 1, -1, -1):
                a, b = rng[dd]
                starts = [s0 + i * self.strides[dd] for i in range(a, b) for s0 in starts]
        blocks = set()
        for s0 in starts:
            lo = (self.off + s0 * self.esz) // BLK
            hi = (self.off + (s0 + L) * self.esz - 1) // BLK
            for b in range(lo, hi + 1):
                blocks.add((self.space, b))
        return View(self.ap[(slice(None),) + idx], blocks)

    def all(self):
        return self[tuple(slice(None) for _ in self.shape)]


class Op:
    __slots__ = ("eng", "fn", "deps", "sig", "sem", "val", "inc", "is_dma", "tag")

    def __init__(self, eng, fn, is_dma):
        self.eng, self.fn, self.is_dma = eng, fn, is_dma
        self.deps = set()
        self.sig = is_dma
        self.sem = None
        self.val = 0
        self.inc = 16 if is_dma else 1


class Prog:
    ENGS = ("pe", "act", "dve", "pool", "sp")
    NDMA_SEM = 12
    NSEM = {"pool": 3}

    def __init__(self):
        self.ops = {e: [] for e in self.ENGS}
        self.state = {}
        self.dma_count = {e: 0 for e in self.ENGS}
        self.dma_last = {}
        self.tag = ""

    def add(self, eng, fn, reads=(), writes=(), dma=False):
        op = Op(eng, fn, dma)
        op.tag = self.tag
        deps = op.deps
        st = self.state
        for v in reads:
            for b in v.blocks:
                e = st.get(b)
                if e is not None and e[0] is not None:
                    deps.add(e[0])
        for v in writes:
            for b in v.blocks:
                e = st.get(b)
                if e is not None:
                    if e[0] is not None:
                        deps.add(e[0])
                    deps.update(e[1])
        for v in reads:
            for b in v.blocks:
                e = st.get(b)
                if e is None:
                    st[b] = [None, [op]]
                else:
                    e[1].append(op)
        for v in writes:
            for b in v.blocks:
                st[b] = [op, []]
        deps.discard(op)
        if dma:
            n = self.dma_count[eng]
            self.dma_count[eng] = n + 1
            nsem = self.NSEM.get(eng, self.NDMA_SEM)
            slot = n % nsem
            prev = self.dma_last.get((eng, slot))
            if prev is not None:
                deps.add(prev)
            self.dma_last[(eng, slot)] = op
            op.sem = (eng, slot)
            op.val = 16 * (n // nsem + 1)
        self.ops[eng].append(op)
        return op

    def finalize(self):
        for e in self.ENGS:
            for op in self.ops[e]:
                for d in op.deps:
                    if d.is_dma:
                        continue
                    if d.eng == "pe" and e == "pe":
                        continue
                    d.sig = True
        for e in self.ENGS:
            cnt = 0
            for op in self.ops[e]:
                if op.is_dma:
                    continue
                if op.sig:
                    cnt += 1
                    op.sem = (e, "c")
                    op.val = cnt

    def emit_engine(self, ename, eng, sems):
        seen = {}
        for op in self.ops[ename]:
            need = {}
            for d in op.deps:
                if (not d.is_dma) and d.eng == "pe" and ename == "pe":
                    continue
                if d.val > need.get(d.sem, 0):
                    need[d.sem] = d.val
            for k, v in need.items():
                if seen.get(k, 0) >= v:
                    continue
                eng.wait_ge(sems[k], v)
                seen[k] = v
            if op.fn is not None:
                ins = op.fn(eng)
                if op.sig:
                    ins.then_inc(sems[op.sem], op.inc)


class Ring:
    def __init__(self, bufs):
        self.bufs = bufs
        self.i = 0

    def next(self):
        b = self.bufs[self.i % len(self.bufs)]
        self.i += 1
        return b


def build(T=SEQ, layers=(0, 1)):
    nc = bass.Bass("TRN2", target_bir_lowering=False)
    ntile = T // TT
    pg = Prog()

    def din(name, shape):
        return nc.dram_tensor(name, list(shape), F32, kind="ExternalInput").ap()

    x_d = din("x", [T, D])
    p_d = din("p", [2, T, DPLE])
    ng_d = din("norm_g", [2, 8, D])
    w_f1g = din("ffn1_w_gate", [2, D, DFF])
    w_f1u = din("ffn1_w_up", [2, D, DFF])
    w_f1d = din("ffn1_w_down", [2, DFF, D])
    w_f2g = din("ffn2_w_gate", [2, D, DFF])
    w_f2u = din("ffn2_w_up", [2, D, DFF])
    w_f2d = din("ffn2_w_down", [2, DFF, D])
    w_cin = din("conv_w_in", [1, D, 3 * D])
    w_cw = din("conv_w", [1, 3, D])
    w_cout = din("conv_w_out", [1, D, D])
    w_rin = din("ret_w_in", [1, D, 2 * H * DK + 2 * H * DV])
    w_rout = din("ret_w_out", [1, H * DV, D])
    w_pp = din("ple_w_proj", [2, DPLE, D])
    w_pg = din("ple_w_gate", [2, D, D])
    c_ident = din("c_ident", [P, P])
    c_mask = din("c_mask", [P, H, P])
    c_dq = din("c_dq", [P, H, P])
    c_dk = din("c_dk", [P, H])
    c_cos = din("c_cos", [P, T])
    c_sin = din("c_sin", [P, T])
    y_d = nc.dram_tensor("y", [T, D], F32, kind="ExternalOutput").ap()
    WSCR_ELEMS = 2 * (2 * 3 * D * DFF) + D * 3 * D + D * D + D * (2 * H * DK + 2 * H * DV) + H * DV * D + 2 * (DPLE * D + D * D)
    wscr = nc.dram_tensor("wscr", [WSCR_ELEMS], BF16, kind="Internal").ap()
    wreg = {}
    wscr_cur = [0]
    cur_tile = [0]

    ARENA_BYTES = 206 * 1024
    arena_cm = nc.sbuf_tensor("arena", [P, ARENA_BYTES // 4], F32)
    psum_cm = nc.psum_tensor("ps", [P, 8, 512], F32)
    arena = arena_cm.__enter__()
    psum = psum_cm.__enter__()

    cursor = [0]

    def carve_at(off, shape, dt):
        esz = 4 if dt == F32 else 2
        n = int(np.prod(shape))
        nbytes = n * esz
        assert off % 4 == 0
        ap = arena[:, off // 4:(off + nbytes + 3) // 4]
        if dt == BF16:
            ap = ap.bitcast(BF16)
        if len(shape) == 2:
            ap = ap.rearrange("p (a b) -> p a b", a=shape[0])
        elif len(shape) == 3:
            ap = ap.rearrange("p (a b c) -> p a b c", a=shape[0], b=shape[1])
        return Buf(ap, "sb", off, shape, esz)

    def carve(shape, dt):
        esz = 4 if dt == F32 else 2
        nbytes = int(np.prod(shape)) * esz
        off = cursor[0]
        cursor[0] = (off + nbytes + BLK - 1) // BLK * BLK
        assert cursor[0] <= ARENA_BYTES, ("arena overflow", cursor[0])
        return carve_at(off, shape, dt)

    X = carve([KC, TT], F32)
    S = carve([H * 2, DV], F32)
    Sb = carve([H * 2, DV], BF16)
    HALO = carve([KC, 2], F32)
    IDF = carve([P], F32)
    IDB = carve([P], BF16)
    ONES_D = carve([P], BF16)
    ONES_V = carve([P], BF16)
    G = carve([P], F32)
    GH = carve([P], F32)
    CW = carve([P], F32)
    MASK = carve([H, P], F32)
    DQ = carve([H, P], F32)
    DKH = carve([H], F32)
    EPSC = carve([1], F32)
    PT = carve([2, 2, TT], BF16)
    CS = carve([2, TT], F32)
    XS = Ring([carve([D], F32) for _ in range(2)])
    PS_ST = Ring([carve([DPLE], F32) for _ in range(2)])
    LOADT = carve([P], F32)
    XN = carve([KC, TT], BF16)
    RSTD = Ring([carve([TT], F32) for _ in range(2)])
    SQ = Ring([carve([TT], BF16) for _ in range(10)])
    TMPA = Ring([carve([TT], F32) for _ in range(3)])
    TMPB = Ring([carve([TT], F32) for _ in range(3)])
    ST_R = Ring([carve([P], BF16) for _ in range(5)])
    KT_R = Ring([carve([2 * P], BF16) for _ in range(5)])
    RSG_R = Ring([carve([P], F32) for _ in range(3)])
    WBYTES = 40 * 1024
    w_base = cursor[0]
    cursor[0] += WBYTES
    scr0 = cursor[0]
    Hh = carve_at(scr0, [FC, TT], BF16)
    Y = carve_at(scr0 + 22 * 1024, [KC, TT], F32)
    U = carve_at(scr0, [KC, TT + 2], F32)
    BV = carve_at(scr0 + 38 * 1024, [KC, TT], BF16)
    YR = carve_at(scr0, [H * 4, TT], BF16)
    Q = carve_at(scr0 + 16 * 1024, [H * 2, TT], BF16)
    QD = carve_at(scr0 + 24 * 1024, [H * 2, TT], BF16)
    Kb = carve_at(scr0 + 32 * 1024, [H * 2, TT], BF16)
    V = carve_at(scr0 + 40 * 1024, [NCH, H * DV], BF16)
    assert scr0 + 56 * 1024 <= ARENA_BYTES, scr0
    OSLOT = [carve_at(scr0 + i * 4096, [D], F32) for i in range(NCH)]
    ISLOT = [carve_at(scr0 + 16 * 1024 + i * 4096, [D], F32) for i in range(NCH)]
    print("sbuf bytes used", scr0 + 56 * 1024)

    PSB = Buf(psum, "ps", 0, [8, 512], 4)
    PSB16 = Buf(psum.bitcast(BF16), "ps", 0, [8, 1024], 2)
    ps_i = [0]

    reserved = [False]

    def ps_next():
        b = ps_i[0] % (4 if reserved[0] else 6)
        ps_i[0] += 1
        return b

    ss_i = [0]

    def ss_next():
        b = 6 + ss_i[0] % 2
        ss_i[0] += 1
        return b

    w_cur = [0]

    def walloc(shape):
        nbytes = int(np.prod(shape)) * 2
        nb = (nbytes + BLK - 1) // BLK * BLK
        assert nb <= WBYTES
        if w_cur[0] + nb > WBYTES:
            w_cur[0] = 0
        off = w_base + w_cur[0]
        w_cur[0] += nb
        return carve_at(off, shape, BF16)

    def mm(out, lhsT, rhs, start, stop):
        return pg.add("pe", lambda t: t.matmul(out.ap, lhsT.ap, rhs.ap, start=start, stop=stop),
                      reads=[lhsT, rhs], writes=[out])

    def warm(n):
        for _ in range(n):
            pg.add("pe", lambda t: t.matmul(PSB[5].ap, ONES_D.all().ap, XN[0].ap, start=True, stop=True),
                   reads=[ONES_D.all()], writes=[])

    def tr(out, in_, ident):
        return pg.add("pe", lambda t: t.transpose(out.ap, in_.ap, ident.ap), reads=[in_, ident], writes=[out])

    def act(out, in_, func, scale=1.0, bias=None, extra_reads=()):
        rd = [in_] + list(extra_reads)
        if bias is not None:
            rd.append(bias)
        sc = scale.ap if isinstance(scale, View) else scale
        if isinstance(scale, View):
            rd.append(scale)
        if bias is not None:
            f = lambda a: a.activation(out.ap, in_.ap, func, bias=bias.ap, scale=sc)
        else:
            f = lambda a: a.activation(out.ap, in_.ap, func, scale=sc)
        return pg.add("act", f, reads=rd, writes=[out])

    def pl(e):
        if e == "pool" and cur_tile[0] == 0:
            return "dve"
        return e

    def tt(out, in0, in1, op, in1_ap=None, eng="dve"):
        a1 = in1.ap if in1_ap is None else in1_ap
        return pg.add(pl(eng), lambda v: v.tensor_tensor(out=out.ap, in0=in0.ap, in1=a1, op=op),
                      reads=[in0, in1], writes=[out])

    def stt(out, in0, scalar, in1, op0, op1, eng="dve"):
        if isinstance(scalar, View):
            return pg.add(pl(eng), lambda v: v.scalar_tensor_tensor(out=out.ap, in0=in0.ap, scalar=scalar.ap, in1=in1.ap,
                                                                    op0=op0, op1=op1),
                          reads=[in0, scalar, in1], writes=[out])
        return pg.add(pl(eng), lambda v: v.scalar_tensor_tensor(out=out.ap, in0=in0.ap, scalar=scalar, in1=in1.ap,
                                                                op0=op0, op1=op1),
                      reads=[in0, in1], writes=[out])

    def ts(out, in0, scalar, op, eng="dve"):
        if isinstance(scalar, View):
            return pg.add(pl(eng), lambda v: v.tensor_scalar(out=out.ap, in0=in0.ap, scalar1=scalar.ap, scalar2=None, op0=op),
                          reads=[in0, scalar], writes=[out])
        return pg.add(pl(eng), lambda v: v.tensor_scalar(out=out.ap, in0=in0.ap, scalar1=scalar, scalar2=None, op0=op),
                      reads=[in0], writes=[out])

    def vcopy(out, in_):
        return pg.add("dve", lambda v: v.tensor_copy(out=out.ap, in_=in_.ap), reads=[in_], writes=[out])

    def vmemset(out, val):
        return pg.add("dve", lambda v: v.memset(out.ap, val), writes=[out])

    def vrecip(out, in_):
        return pg.add("dve", lambda v: v.reciprocal(out=out.ap, in_=in_.ap), reads=[in_], writes=[out])

    def dma_in(eng, out, src_ap):
        if eng == "pool":
            return pg.add("pool", lambda g: g.dma_start(out=out.ap, in_=src_ap), writes=[out], dma=True)
        return pg.add("sp", lambda s: s.dma_start(out=out.ap, in_=src_ap), writes=[out], dma=True)

    def dma_out(dst_ap, src):
        return pg.add("act", lambda s: s.dma_start(out=dst_ap, in_=src.ap), reads=[src], dma=True)

    def wload(src_ap, shape, key):
        n = P * int(np.prod(shape))
        if key not in wreg:
            off = wscr_cur[0]
            wscr_cur[0] += n
            assert wscr_cur[0] <= WSCR_ELEMS
            if len(shape) == 2:
                dst = wscr[off:off + n].rearrange("(p k c) -> p k c", p=P, k=shape[0])
            else:
                dst = wscr[off:off + n].rearrange("(p c) -> p c", p=P)
            cops = []
            nk = shape[0]
            step = 8 if nk > 8 else nk
            for k0 in range(0, nk, step):
                k1 = min(nk, k0 + step)
                d_ = dst[:, k0:k1]
                s_ = src_ap[:, k0:k1]
                cops.append(pg.add("pool", lambda g, d_=d_, s_=s_: g.dma_start(out=d_, in_=s_), dma=True))
            wreg[key] = (off, cops, dst)
        off, cops, dst = wreg[key]
        wb = walloc(shape)
        op = dma_in("sp", wb.all(), dst)
        op.deps.update(cops)
        return wb

    def wview(w, l, c0, c1):
        return w[l].rearrange("(k p) n -> p k n", p=P)[:, :, c0:c1]

    vmemset(S.all(), 0.0)
    vmemset(Sb.all(), 0.0)
    vmemset(HALO.all(), 0.0)
    vmemset(EPSC.all(), EPS)
    vmemset(LOADT.all(), 0.0)
    dma_in("sp", IDF.all(), c_ident)
    dma_in("pool", IDB.all(), c_ident)
    dma_in("sp", MASK.all(), c_mask)
    dma_in("sp", DQ.all(), c_dq)
    dma_in("sp", DKH.all(), c_dk)
    t_ones = TMPA.next()
    vmemset(t_ones[0:P], 1.0 / D)
    vcopy(ONES_D.all(), t_ones[0:P])
    t_ones = TMPA.next()
    vmemset(t_ones[0:P], 1.0 / DV)
    vcopy(ONES_V.all(), t_ones[0:P])
    xs0 = XS.next()
    dma_in("sp", xs0[0:P], ng_d.rearrange("l n (c p) -> (l n c) p", p=P))
    b = ps_next()
    tr(PSB[b, 0:P], xs0[0:P], IDF.all())
    vcopy(G.all(), PSB[b, 0:P])
    ts(GH.all(), G.all(), 0.5, ALU.mult)
    dma_in("sp", Buf(LOADT.ap[0:24], "sb", LOADT.off, [P], 4).all(), w_cw[0].rearrange("k (c p) -> (k c) p", p=P))
    b = ps_next()
    tr(PSB[b, 0:P], LOADT.all(), IDF.all())
    vcopy(CW.all(), PSB[b, 0:P])

    def gcol(Gb, l, n, c):
        j = (l * 8 + n) * 8 + c
        return Gb[j:j + 1]

    def rstd_from(ssb):
        r = RSTD.next()
        act(r.all(), PSB[ssb], AF.Sqrt, bias=EPSC.all())
        vrecip(r.all(), r.all())
        return r

    def prenorm(l, n):
        pg.tag = pg.tag.split('.')[0] + '.pre'
        ssb = ss_next()
        for c in range(KC):
            sq = SQ.next()
            act(sq.all(), X[c], AF.Square)
            mm(PSB[ssb], ONES_D.all(), sq.all(), c == 0, c == KC - 1)
        r = rstd_from(ssb)
        for c in range(KC):
            if c >= 5 and cur_tile[0] > 0:
                tmp = TMPB.next()
                tt(tmp.all(), X[c], r.all(), ALU.mult, eng="pool")
                act(XN[c], tmp.all(), AF.Copy, scale=gcol(G, l, n, c))
            else:
                stt(XN[c], X[c], gcol(G, l, n, c), r.all(), ALU.mult, ALU.mult)

    class PostNorm:
        def __init__(self, l, n, half, nxt):
            self.acc = (4, 5, 6, 7)
            reserved[0] = True
            self.n = 0
            self.pending = []
            self.l, self.nn = l, n
            self.Gb = GH if half else G
            self.nxt = nxt
            self.full = nxt is not None

        def _flush(self):
            if self.pending:
                for a_, sq in self.pending:
                    mm(PSB[self.acc[a_]], ONES_D.all(), sq.all(), self.n == 0, self.n == KC - 1)
                self.pending = []
                self.n += 1

        def _extra(self, j, pend):
            if self.full:
                s1 = SQ.next()
                act(s1.all(), Y[j], AF.Square)
                pend.append((1, s1))
                s2 = SQ.next()
                tt(s2.all(), X[j], Y[j], ALU.mult, eng="pool")
                pend.append((2, s2))
                s0 = SQ.next()
                act(s0.all(), X[j], AF.Square)
                pend.append((0, s0))

        def add_from_psum(self, j, psv):
            self._flush()
            act(Y[j], psv, AF.Copy, scale=gcol(self.Gb, self.l, self.nn, j))
            sq = SQ.next()
            act(sq.all(), psv, AF.Square)
            pend = [(3, sq)]
            self._extra(j, pend)
            self.pending = pend

        def add_from_y(self, j):
            self._flush()
            sq = SQ.next()
            act(sq.all(), Y[j], AF.Square)
            pend = [(3, sq)]
            act(Y[j], Y[j], AF.Copy, scale=gcol(self.Gb, self.l, self.nn, j))
            self._extra(j, pend)
            self.pending = pend

        def finish(self):
            self._flush()
            assert self.n == KC
            reserved[0] = False
            pg.tag = pg.tag.split('.')[0] + '.post'
            r = rstd_from(self.acc[3])
            if not self.full:
                for j in range(KC):
                    t = TMPA.next()
                    e = "pool" if j in (1, 4, 7) else "dve"
                    tt(t.all(), Y[j], r.all(), ALU.mult, eng=e)
                    tt(X[j], t.all(), X[j], ALU.add, eng=e)
                return
            l2, n2 = self.nxt
            m = TMPB.next()
            tt(m.all(), PSB[self.acc[1]], r.all(), ALU.mult)
            stt(m.all(), PSB[self.acc[2]], 2.0, m.all(), ALU.mult, ALU.add)
            tt(m.all(), m.all(), r.all(), ALU.mult)
            tt(m.all(), m.all(), PSB[self.acc[0]], ALU.add)
            r2 = RSTD.next()
            act(r2.all(), m.all(), AF.Sqrt, bias=EPSC.all())
            vrecip(r2.all(), r2.all())
            for j in range(KC):
                t = TMPA.next()
                e = "pool" if j in (2, 4, 6) else "dve"
                tt(t.all(), Y[j], r.all(), ALU.mult, eng=e)
                tt(X[j], t.all(), X[j], ALU.add, eng=e)
                stt(XN[j], X[j], gcol(G, l2, n2, j), r2.all(), ALU.mult, ALU.mult)

    def ffn(l, wg, wu, wd, n_pre, n_post, tag, pre, nxt):
        pg.tag = tag
        if pre:
            prenorm(l, n_pre)
        pg.tag = tag + '.gu'
        tiles_ = {}

        def gu_tile(f2):
            if f2 not in tiles_:
                tiles_[f2] = (wload(wview(wg, l, f2 * 256, (f2 + 1) * 256), [KC, 256], (tag, 'g', l, f2)),
                              wload(wview(wu, l, f2 * 256, (f2 + 1) * 256), [KC, 256], (tag, 'u', l, f2)))
            return tiles_[f2]

        def gu_evac(f, bg, bu):
            sg = TMPB.next()
            act(sg.all(), PSB[bg], AF.Silu)
            tt(Hh[f], sg.all(), PSB[bu], ALU.mult)

        head = [(0, 0), (0, 1), (1, 0)]
        hb_ = []
        for (f2, hf) in head:
            gu_tile(f2)
            hb_.append((ps_next(), ps_next()))
        for k in range(KC):
            for (f2, hf), (bg, bu) in zip(head, hb_):
                g_t, u_t = tiles_[f2]
                mm(PSB[bg], g_t[k, hf * P:(hf + 1) * P], XN[k], k == 0, k == KC - 1)
                mm(PSB[bu], u_t[k, hf * P:(hf + 1) * P], XN[k], k == 0, k == KC - 1)
        for (f2, hf), (bg, bu) in zip(head, hb_):
            gu_evac(2 * f2 + hf, bg, bu)
        for f2 in range(FC // 2):
            for hf in range(2):
                if (f2, hf) in head:
                    continue
                g_t, u_t = gu_tile(f2)
                bg, bu = ps_next(), ps_next()
                for k in range(KC):
                    mm(PSB[bg], g_t[k, hf * P:(hf + 1) * P], XN[k], k == 0, k == KC - 1)
                for k in range(KC):
                    mm(PSB[bu], u_t[k, hf * P:(hf + 1) * P], XN[k], k == 0, k == KC - 1)
                gu_evac(2 * f2 + hf, bg, bu)
        pg.tag = tag + '.dn'
        pn = PostNorm(l, n_post, True, nxt)
        for j2 in range(KC // 2):
            d_t = wload(wd[l].rearrange("(k p) n -> p k n", p=P)[:, :, j2 * 256:(j2 + 1) * 256], [FC, 256], (tag, 'd', l, j2))
            for hf in range(2):
                j = 2 * j2 + hf
                by = ps_next()
                for f in range(FC):
                    mm(PSB[by], d_t[f, hf * P:(hf + 1) * P], Hh[f], f == 0, f == FC - 1)
                pn.add_from_psum(j, PSB[by])
        pn.finish()

    def conv_mixer(l, pre, nxt):
        pg.tag = 'conv'
        if pre:
            prenorm(l, MIX_PRE)
        pg.tag = 'conv.in'
        for m2 in range(KC // 2):
            wb_ = wload(wview(w_cin, 0, m2 * 256, (m2 + 1) * 256), [KC, 256], ('cb', m2))
            wc_ = wload(wview(w_cin, 0, D + m2 * 256, D + (m2 + 1) * 256), [KC, 256], ('cc', m2))
            wh_ = wload(wview(w_cin, 0, 2 * D + m2 * 256, 2 * D + (m2 + 1) * 256), [KC, 256], ('ch', m2))
            for hf in range(2):
                m = 2 * m2 + hf
                bb, bc, bh = ps_next(), ps_next(), ps_next()
                for k in range(KC):
                    mm(PSB[bc], wc_[k, hf * P:(hf + 1) * P], XN[k], k == 0, k == KC - 1)
                for k in range(KC):
                    mm(PSB[bh], wh_[k, hf * P:(hf + 1) * P], XN[k], k == 0, k == KC - 1)
                for k in range(KC):
                    mm(PSB[bb], wb_[k, hf * P:(hf + 1) * P], XN[k], k == 0, k == KC - 1)
                cs = TMPB.next()
                act(cs.all(), PSB[bc], AF.Copy)
                bsb = TMPB.next()
                act(bsb.all(), PSB[bb], AF.Copy)
                vcopy(U[m, 0:2], HALO[m])
                tt(U[m, 2:TT + 2], cs.all(), PSB[bh], ALU.mult)
                v = TMPA.next()
                act(v.all(), U[m, 2:TT + 2], AF.Copy, scale=CW[16 + m:17 + m])
                stt(v.all(), U[m, 1:TT + 1], CW[8 + m:9 + m], v.all(), ALU.mult, ALU.add)
                stt(v.all(), U[m, 0:TT], CW[m:m + 1], v.all(), ALU.mult, ALU.add)
                tt(BV[m], v.all(), bsb.all(), ALU.mult, eng="pool")
                vcopy(HALO[m], U[m, TT:TT + 2])
        pg.tag = 'conv.out'
        pn = PostNorm(l, MIX_POST, False, nxt)
        for j2 in range(KC // 2):
            o_t = wload(wview(w_cout, 0, j2 * 256, (j2 + 1) * 256), [KC, 256], ('co', j2))
            for hf in range(2):
                j = 2 * j2 + hf
                by = ps_next()
                for m in range(KC):
                    mm(PSB[by], o_t[m, hf * P:(hf + 1) * P], BV[m], m == 0, m == KC - 1)
                pn.add_from_psum(j, PSB[by])
        pn.finish()

    GAMMA = [1.0 - 2.0 ** (-5 - h) for h in range(H)]
    GAMMA_C = [float(np.float32(g) ** np.float32(CH)) for g in GAMMA]

    def retention(l, pre, nxt):
        pg.tag = 'ret'
        if pre:
            prenorm(l, MIX_PRE)
        pg.tag = 'ret.qk'
        qoff, koff, voff, goff = 0, H * DK, 2 * H * DK, 2 * H * DK + H * DV
        for h in range(H):
            for which in range(2):
                base = (qoff if which == 0 else koff) + h * DK
                w_t = wload(wview(w_rin, 0, base, base + DK), [KC, DK], ('rqk', h, which))
                b1, b2 = ps_next(), ps_next()
                for k in range(KC):
                    mm(PSB[b1], w_t[k, 0:P], XN[k], k == 0, k == KC - 1)
                for k in range(KC):
                    mm(PSB[b2], w_t[k, P:2 * P], XN[k], k == 0, k == KC - 1)
                dst = Q if which == 0 else Kb
                a = TMPA.next()
                bt = TMPB.next()
                tt(a.all(), PSB[b1], CS[0], ALU.mult)
                tt(bt.all(), PSB[b2], CS[1], ALU.mult)
                tt(dst[2 * h], a.all(), bt.all(), ALU.subtract, eng="pool")
                a = TMPA.next()
                bt = TMPB.next()
                tt(a.all(), PSB[b2], CS[0], ALU.mult)
                tt(bt.all(), PSB[b1], CS[1], ALU.mult)
                tt(dst[2 * h + 1], a.all(), bt.all(), ALU.add, eng="pool")
                if which == 0:
                    dq_b = DQ[h].ap.unsqueeze(1).to_broadcast([P, NCH, P])
                    for hh in range(2):
                        qd3 = QD[2 * h + hh].ap.rearrange("p (a b) -> p a b", a=NCH)
                        q3 = Q[2 * h + hh].ap.rearrange("p (a b) -> p a b", a=NCH)
                        pg.add(pl("pool"), lambda v, qd3=qd3, q3=q3, dq_b=dq_b: v.tensor_tensor(out=qd3, in0=q3, in1=dq_b, op=ALU.mult),
                               reads=[Q[2 * h + hh], DQ[h]], writes=[QD[2 * h + hh]])
        pg.tag = 'ret.v'
        for vb in range(H):
            w_t = wload(wview(w_rin, 0, voff + vb * DV, voff + (vb + 1) * DV), [KC, DV], ('rv', vb))
            for n in range(NCH):
                bv_ = ps_next()
                for k in range(KC):
                    mm(PSB[bv_], XN[k, n * P:(n + 1) * P], w_t[k], k == 0, k == KC - 1)
                act(V[n, vb * DV:(vb + 1) * DV], PSB[bv_], AF.Copy)
        pg.tag = 'ret.core'
        for n in range(NCH):
            tsl = slice(n * P, (n + 1) * P)
            sts, kts, bos, sqs = [], [], [], []
            for h in range(H):
                bs = ps_next()
                for dc in range(2):
                    mm(PSB[bs, 0:P], Kb[2 * h + dc, tsl], Q[2 * h + dc, tsl], dc == 0, dc == 1)
                for dc in range(2):
                    tr(PSB16[bs, 512 + dc * P:512 + (dc + 1) * P], Kb[2 * h + dc, tsl], IDB.all())
                st_ = ST_R.next()
                s_ap = PSB[bs, 0:P].ap
                pg.add("dve", lambda v, o=st_.all().ap, i0=s_ap, i1=MASK[h].ap: v.tensor_tensor(out=o, in0=i0, in1=i1, op=ALU.mult),
                       reads=[PSB[bs], MASK[h]], writes=[st_.all()])
                kt = KT_R.next()
                k_ap = PSB16[bs, 512:512 + 2 * P].ap
                pg.add("dve", lambda v, o=kt.all().ap, i0=k_ap, sc=DKH[h:h + 1].ap: v.tensor_scalar(out=o, in0=i0, scalar1=sc, scalar2=None, op0=ALU.mult),
                       reads=[PSB[bs], DKH[h:h + 1]], writes=[kt.all()])
                sts.append(st_)
                kts.append(kt)
            for h in range(H):
                bo = ps_next()
                for ec in range(4):
                    osl = slice(ec * P, (ec + 1) * P)
                    mm(PSB[bo, osl], V[n, h * DV + ec * P:h * DV + (ec + 1) * P], sts[h].all(), True, False)
                    for dc in range(2):
                        mm(PSB[bo, osl], Sb[2 * h + dc, ec * P:(ec + 1) * P], QD[2 * h + dc, tsl], False, dc == 1)
                sq = SQ.next()
                act(sq.all(), PSB[bo], AF.Square)
                bos.append(bo)
                sqs.append(sq)
            for h in range(H):
                bo, sq = bos[h], sqs[h]
                bgn = ss_next()
                for ec in range(4):
                    mm(PSB[bgn, 0:P], ONES_V.all(), sq[ec * P:(ec + 1) * P], ec == 0, ec == 3)
                rs = RSG_R.next()
                act(rs.all(), PSB[bgn, 0:P], AF.Sqrt, bias=EPSC.all())
                vrecip(rs.all(), rs.all())
                rs_b = rs.all().ap.unsqueeze(1).to_broadcast([P, 4, P])
                o3 = PSB[bo].ap.rearrange("p (a b) -> p a b", a=4)
                yr3 = YR[4 * h:4 * h + 4, tsl]
                pg.add("dve", lambda v, yr3=yr3, o3=o3, rs_b=rs_b: v.tensor_tensor(out=yr3.ap, in0=o3, in1=rs_b, op=ALU.mult),
                       reads=[PSB[bo], rs.all()], writes=[yr3])
            for h in range(H):
                for dc in range(2):
                    bd = ps_next()
                    mm(PSB[bd], kts[h][dc * P:(dc + 1) * P], V[n, h * DV:(h + 1) * DV], True, True)
                    stt(S[2 * h + dc], S[2 * h + dc], GAMMA_C[h], PSB[bd], ALU.mult, ALU.add)
                    act(Sb[2 * h + dc], S[2 * h + dc], AF.Copy)
        pg.tag = 'ret.g'
        for gb in range(H * DV // 256):
            w_t = wload(wview(w_rin, 0, goff + gb * 256, goff + (gb + 1) * 256), [KC, 256], ('rg', gb))
            for hf in range(2):
                c = 2 * gb + hf
                bg = ps_next()
                for k in range(KC):
                    mm(PSB[bg], w_t[k, hf * P:(hf + 1) * P], XN[k], k == 0, k == KC - 1)
                sgt = TMPB.next()
                act(sgt.all(), PSB[bg], AF.Silu)
                tt(YR[c], YR[c], sgt.all(), ALU.mult)
        pg.tag = 'ret.out'
        pn = PostNorm(l, MIX_POST, False, nxt)
        for j2 in range(KC // 2):
            o_t = wload(wview(w_rout, 0, j2 * 256, (j2 + 1) * 256), [H * 4, 256], ('ro', j2))
            for hf in range(2):
                j = 2 * j2 + hf
                by = ps_next()
                for c in range(H * 4):
                    mm(PSB[by], o_t[c, hf * P:(hf + 1) * P], YR[c], c == 0, c == H * 4 - 1)
                pn.add_from_psum(j, PSB[by])
        pn.finish()

    def ple(l, pre, nxt):
        pg.tag = 'ple'
        if pre:
            prenorm(l, PLE_PRE)
        pg.tag = 'ple.main'
        pn = PostNorm(l, PLE_POST, False, nxt)
        for j2 in range(KC // 2):
            g_t = wload(wview(w_pg, l, j2 * 256, (j2 + 1) * 256), [KC, 256], ('pg', l, j2))
            p_t = wload(wview(w_pp, l, j2 * 256, (j2 + 1) * 256), [2, 256], ('pp', l, j2))
            for hf in range(2):
                j = 2 * j2 + hf
                bg, be = ps_next(), ps_next()
                for k in range(KC):
                    mm(PSB[bg], g_t[k, hf * P:(hf + 1) * P], XN[k], k == 0, k == KC - 1)
                for k in range(2):
                    mm(PSB[be], p_t[k, hf * P:(hf + 1) * P], PT[l, k], k == 0, k == 1)
                sg = TMPB.next()
                act(sg.all(), PSB[bg], AF.Sigmoid)
                tt(Y[j], sg.all(), PSB[be], ALU.mult)
                pn.add_from_y(j)
        pn.finish()

    out_dmas = []

    def load_tile(ti):
        pg.tag = 'load'
        t0 = ti * TT
        for n in range(NCH):
            dma_in("sp", ISLOT[n].all(), x_d[t0 + n * P:t0 + (n + 1) * P, :])
        for n in range(NCH):
            xs = ISLOT[n]
            for hb in range(2):
                b = ps_next()
                for q in range(4):
                    c = hb * 4 + q
                    tr(PSB[b, q * P:(q + 1) * P], xs[c * P:(c + 1) * P], IDF.all())
                dst = X[hb * 4:hb * 4 + 4, n * P:(n + 1) * P]
                src = PSB[b].ap.rearrange("p (a b) -> p a b", a=4)
                pg.add("act", lambda a, dst=dst, src=src: a.activation(dst.ap, src, AF.Copy),
                       reads=[PSB[b]], writes=[dst])
            for l in layers:
                pst = PS_ST.next()
                dma_in("sp", pst.all(), p_d[l, t0 + n * P:t0 + (n + 1) * P, :])
                b = ps_next()
                for k in range(2):
                    tr(PSB[b, k * P:(k + 1) * P], pst[k * P:(k + 1) * P], IDF.all())
                dst = PT[l, 0:2, n * P:(n + 1) * P]
                src = PSB[b, 0:2 * P].ap.rearrange("p (a b) -> p a b", a=2)
                pg.add("dve", lambda v, dst=dst, src=src: v.tensor_copy(out=dst.ap, in_=src),
                       reads=[PSB[b, 0:2 * P]], writes=[dst])
        if 1 in layers:
            dma_in("sp", CS[0], c_cos[:, t0:t0 + TT])
            dma_in("sp", CS[1], c_sin[:, t0:t0 + TT])

    def store_tile(ti):
        pg.tag = 'store'
        t0 = ti * TT
        for n in range(NCH):
            xs = OSLOT[n]
            for hb in range(2):
                b = ps_next()
                for q in range(4):
                    c = hb * 4 + q
                    tr(PSB[b, q * P:(q + 1) * P], X[c, n * P:(n + 1) * P], IDF.all())
                act(xs[hb * 4 * P:(hb * 4 + 4) * P], PSB[b], AF.Copy)
            out_dmas.append(dma_out(y_d[t0 + n * P:t0 + (n + 1) * P, :], xs.all()))

    for ti in range(ntile):
        cur_tile[0] = ti
        load_tile(ti)
        steps = []
        for l in layers:
            steps += [("f1", l, FFN1_PRE), ("mix", l, MIX_PRE), ("f2", l, FFN2_PRE), ("ple", l, PLE_PRE)]
        for i, (kind, l, _) in enumerate(steps):
            pre = (i == 0)
            nxt = (steps[i + 1][1], steps[i + 1][2]) if i + 1 < len(steps) else None
            if kind == "f1":
                ffn(l, w_f1g, w_f1u, w_f1d, FFN1_PRE, FFN1_POST, 'f1', pre, nxt)
            elif kind == "mix":
                if l % 2 == 0:
                    conv_mixer(l, pre, nxt)
                else:
                    retention(l, pre, nxt)
            elif kind == "f2":
                ffn(l, w_f2g, w_f2u, w_f2d, FFN2_PRE, FFN2_POST, 'f2', pre, nxt)
            else:
                ple(l, pre, nxt)
        store_tile(ti)
    fin = pg.add("sp", None)
    fin.deps.update(out_dmas)

    pg.finalize()

    sem_cms = []
    sems = {}

    def mksem(key, name):
        cm = nc.semaphore(name)
        sem_cms.append(cm)
        sems[key] = cm.__enter__()

    for e in ("pe", "act", "dve", "pool", "sp"):
        mksem((e, "c"), "c_" + e)
    for e in ("pool", "sp", "act"):
        for s_ in range(Prog.NDMA_SEM):
            mksem((e, s_), "d_%s_%d" % (e, s_))

    with nc.Block() as block:
        @block.tensor
        def _(t):
            pg.emit_engine("pe", t, sems)

        @block.scalar
        def _(a):
            pg.emit_engine("act", a, sems)

        @block.vector
        def _(v):
            pg.emit_engine("dve", v, sems)

        @block.gpsimd
        def _(g):
            pg.emit_engine("pool", g, sems)

        @block.sync
        def _(s):
            pg.emit_engine("sp", s, sems)

    for cm in reversed(sem_cms):
        cm.__exit__(None, None, None)
    psum_cm.__exit__(None, None, None)
    arena_cm.__exit__(None, None, None)
    nstat = {e: len(pg.ops[e]) for e in Prog.ENGS}
    nstat["_pe_tags"] = [op.tag for op in pg.ops["pe"]]
    return nc, nstat


def make_consts(T):
    f32 = np.float32
    ident = np.eye(P, dtype=f32)
    gam = np.array([1.0 - 2.0 ** (-5 - h) for h in range(H)], dtype=np.float64)
    idx = np.arange(CH, dtype=np.float64)
    diff = idx[None, :] - idx[:, None]
    mask = np.zeros((P, H, P), dtype=f32)
    for h in range(H):
        m = np.where(diff >= 0, gam[h] ** np.maximum(diff, 0.0), 0.0) * (DK ** -0.5)
        mask[:, h, :] = m.astype(f32)
    dq = np.zeros((P, H, P), dtype=f32)
    for h in range(H):
        row = gam[h] ** (idx + 1.0)
        dq[:, h, :] = row.astype(f32)[None, :]
    dk = np.zeros((P, H), dtype=f32)
    for h in range(H):
        dk[:, h] = (gam[h] ** (CH - 1.0 - idx) * (DK ** -0.5)).astype(f32)
    half = DK // 2
    inv_freq = (1.0 / (np.float32(10000.0) ** np.linspace(0.0, 1.0, half, dtype=f32))).astype(f32)
    pos = np.arange(T, dtype=f32)
    ang = (inv_freq[:, None] * pos[None, :]).astype(f32)
    return {
        "c_ident": ident, "c_mask": mask, "c_dq": dq, "c_dk": dk,
        "c_cos": np.cos(ang).astype(f32), "c_sin": np.sin(ang).astype(f32),
    }


_WNAMES = ["norm_g", "ffn1_w_gate", "ffn1_w_up", "ffn1_w_down", "ffn2_w_gate", "ffn2_w_up", "ffn2_w_down",
           "conv_w_in", "conv_w", "conv_w_out", "ret_w_in", "ret_w_out", "ple_w_proj", "ple_w_gate"]


def run(inputs, T, n_cores, layers=(0, 1), trace=False):
    nc, _ = build(T, layers)
    consts = make_consts(T)
    shared = {k: np.ascontiguousarray(np.asarray(inputs[k], dtype=np.float32)) for k in _WNAMES}
    shared.update(consts)
    x = np.asarray(inputs["x"], dtype=np.float32)
    p = np.asarray(inputs["p"], dtype=np.float32)
    in_maps = []
    for c in range(n_cores):
        m = dict(shared)
        m["x"] = np.ascontiguousarray(x[c])
        m["p"] = np.ascontiguousarray(p[:, c])
        in_maps.append(m)
    res = run_bass_kernel_spmd(nc, in_maps, core_ids=list(range(n_cores)), trace=trace)
    out = np.stack([np.asarray(r["y"]) for r in res.results], axis=0)
    return out, res


def kernel(**inputs):
    out, _ = run(inputs, SEQ, N_CORES)
    return out.astype(np.float32)
```

```python
import numpy as np
import concourse.bass as bass
import concourse.mybir as mybir
from concourse.bass_utils import run_bass_kernel_spmd

F32 = mybir.dt.float32
BF16 = mybir.dt.bfloat16
AF = mybir.ActivationFunctionType
ALU = mybir.AluOpType

P = 128
D = 1024
KC = D // P
DFF = 2816
FC = DFF // P
DPLE = 256
TT = 512
NCH = TT // P
EPS = 1e-6
H = 4
DK = 256
DV = 512
CH = 128
N_CORES = 8
SEQ = 4096
BLK = 256

FFN1_PRE, FFN1_POST, MIX_PRE, MIX_POST, FFN2_PRE, FFN2_POST, PLE_PRE, PLE_POST = range(8)


class View:
    __slots__ = ("ap", "blocks")

    def __init__(self, ap, blocks):
        self.ap = ap
        self.blocks = blocks


class Buf:
    def __init__(self, ap, space, off, shape, esz):
        self.ap, self.space, self.off, self.shape, self.esz = ap, space, off, tuple(shape), esz
        st, acc = [], 1
        for n in reversed(self.shape):
            st.append(acc)
            acc *= n
        self.strides = tuple(reversed(st))
        self.nbytes = acc * esz

    def __getitem__(self, idx):
        if not isinstance(idx, tuple):
            idx = (idx,)
        idx = idx + (slice(None),) * (len(self.shape) - len(idx))
        rng = []
        for i, n in zip(idx, self.shape):
            if isinstance(i, int):
                rng.append((i, i + 1))
            else:
                a, b, s = i.indices(n)
                assert s == 1
                rng.append((a, b))
        L = 1
        d = len(rng) - 1
        while d >= 0:
            a, b = rng[d]
            if a == 0 and b == self.shape[d]:
                L *= self.shape[d]
                d -= 1
                continue
            break
        starts = [0]
        if d >= 0:
            a, b = rng[d]
            L *= (b - a)
            starts = [a * self.strides[d]]
            for dd in range(d - 1, -1, -1):
                a, b = rng[dd]
                starts = [s0 + i * self.strides[dd] for i in range(a, b) for s0 in starts]
        blocks = set()
        for s0 in starts:
            lo = (self.off + s0 * self.esz) // BLK
            hi = (self.off + (s0 + L) * self.esz - 1) // BLK
            for b in range(lo, hi + 1):
                blocks.add((self.space, b))
        return View(self.ap[(slice(None),) + idx], blocks)

    def all(self):
        return self[tuple(slice(None) for _ in self.shape)]


class Op:
    __slots__ = ("eng", "fn", "deps", "sig", "sem", "val", "inc", "is_dma", "tag")

    def __init__(self, eng, fn, is_dma):
        self.eng, self.fn, self.is_dma = eng, fn, is_dma
        self.deps = set()
        self.sig = is_dma
        self.sem = None
        self.val = 0
        self.inc = 16 if is_dma else 1


class Prog:
    ENGS = ("pe", "act", "dve", "pool", "sp")
    NDMA_SEM = 12
    NSEM = {"pool": 3}

    def __init__(self):
        self.ops = {e: [] for e in self.ENGS}
        self.state = {}
        self.dma_count = {e: 0 for e in self.ENGS}
        self.dma_last = {}
        self.tag = ""

    def add(self, eng, fn, reads=(), writes=(), dma=False):
        op = Op(eng, fn, dma)
        op.tag = self.tag
        deps = op.deps
        st = self.state
        for v in reads:
            for b in v.blocks:
                e = st.get(b)
                if e is not None and e[0] is not None:
                    deps.add(e[0])
        for v in writes:
            for b in v.blocks:
                e = st.get(b)
                if e is not None:
                    if e[0] is not None:
                        deps.add(e[0])
                    deps.update(e[1])
        for v in reads:
            for b in v.blocks:
                e = st.get(b)
                if e is None:
                    st[b] = [None, [op]]
                else:
                    e[1].append(op)
        for v in writes:
            for b in v.blocks:
                st[b] = [op, []]
        deps.discard(op)
        if dma:
            n = self.dma_count[eng]
            self.dma_count[eng] = n + 1
            nsem = self.NSEM.get(eng, self.NDMA_SEM)
            slot = n % nsem
            prev = self.dma_last.get((eng, slot))
            if prev is not None:
                deps.add(prev)
            self.dma_last[(eng, slot)] = op
            op.sem = (eng, slot)
            op.val = 16 * (n // nsem + 1)
        self.ops[eng].append(op)
        return op

    def finalize(self):
        for e in self.ENGS:
            for op in self.ops[e]:
                for d in op.deps:
                    if d.is_dma:
                        continue
                    if d.eng == "pe" and e == "pe":
                        continue
                    d.sig = True
        for e in self.ENGS:
            cnt = 0
            for op in self.ops[e]:
                if op.is_dma:
                    continue
                if op.sig:
                    cnt += 1
                    op.sem = (e, "c")
                    op.val = cnt

    def emit_engine(self, ename, eng, sems):
        seen = {}
        for op in self.ops[ename]:
            need = {}
            for d in op.deps:
                if (not d.is_dma) and d.eng == "pe" and ename == "pe":
                    continue
                if d.val > need.get(d.sem, 0):
                    need[d.sem] = d.val
            for k, v in need.items():
                if seen.get(k, 0) >= v:
                    continue
                eng.wait_ge(sems[k], v)
                seen[k] = v
            if op.fn is not None:
                ins = op.fn(eng)
                if op.sig:
                    ins.then_inc(sems[op.sem], op.inc)


class Ring:
    def __init__(self, bufs):
        self.bufs = bufs
        self.i = 0

    def next(self):
        b = self.bufs[self.i % len(self.bufs)]
        self.i += 1
        return b


def build(T=SEQ, layers=(0, 1)):
    nc = bass.Bass("TRN2", target_bir_lowering=False)
    ntile = T // TT
    pg = Prog()

    def din(name, shape):
        return nc.dram_tensor(name, list(shape), F32, kind="ExternalInput").ap()

    x_d = din("x", [T, D])
    p_d = din("p", [2, T, DPLE])
    ng_d = din("norm_g", [2, 8, D])
    w_f1g = din("ffn1_w_gate", [2, D, DFF])
    w_f1u = din("ffn1_w_up", [2, D, DFF])
    w_f1d = din("ffn1_w_down", [2, DFF, D])
    w_f2g = din("ffn2_w_gate", [2, D, DFF])
    w_f2u = din("ffn2_w_up", [2, D, DFF])
    w_f2d = din("ffn2_w_down", [2, DFF, D])
    w_cin = din("conv_w_in", [1, D, 3 * D])
    w_cw = din("conv_w", [1, 3, D])
    w_cout = din("conv_w_out", [1, D, D])
    w_rin = din("ret_w_in", [1, D, 2 * H * DK + 2 * H * DV])
    w_rout = din("ret_w_out", [1, H * DV, D])
    w_pp = din("ple_w_proj", [2, DPLE, D])
    w_pg = din("ple_w_gate", [2, D, D])
    c_ident = din("c_ident", [P, P])
    c_mask = din("c_mask", [P, H, P])
    c_dq = din("c_dq", [P, H, P])
    c_dk = din("c_dk", [P, H])
    c_cos = din("c_cos", [P, T])
    c_sin = din("c_sin", [P, T])
    y_d = nc.dram_tensor("y", [T, D], F32, kind="ExternalOutput").ap()
    WSCR_ELEMS = 2 * (2 * 3 * D * DFF) + D * 3 * D + D * D + D * (2 * H * DK + 2 * H * DV) + H * DV * D + 2 * (DPLE * D + D * D)
    wscr = nc.dram_tensor("wscr", [WSCR_ELEMS], BF16, kind="Internal").ap()
    wreg = {}
    wscr_cur = [0]
    cur_tile = [0]

    ARENA_BYTES = 206 * 1024
    arena_cm = nc.sbuf_tensor("arena", [P, ARENA_BYTES // 4], F32)
    psum_cm = nc.psum_tensor("ps", [P, 8, 512], F32)
    arena = arena_cm.__enter__()
    psum = psum_cm.__enter__()

    cursor = [0]

    def carve_at(off, shape, dt):
        esz = 4 if dt == F32 else 2
        n = int(np.prod(shape))
        nbytes = n * esz
        assert off % 4 == 0
        ap = arena[:, off // 4:(off + nbytes + 3) // 4]
        if dt == BF16:
            ap = ap.bitcast(BF16)
        if len(shape) == 2:
            ap = ap.rearrange("p (a b) -> p a b", a=shape[0])
        elif len(shape) == 3:
            ap = ap.rearrange("p (a b c) -> p a b c", a=shape[0], b=shape[1])
        return Buf(ap, "sb", off, shape, esz)

    def carve(shape, dt):
        esz = 4 if dt == F32 else 2
        nbytes = int(np.prod(shape)) * esz
        off = cursor[0]
        cursor[0] = (off + nbytes + BLK - 1) // BLK * BLK
        assert cursor[0] <= ARENA_BYTES, ("arena overflow", cursor[0])
        return carve_at(off, shape, dt)

    X = carve([KC, TT], F32)
    S = carve([H * 2, DV], F32)
    Sb = carve([H * 2, DV], BF16)
    HALO = carve([KC, 2], F32)
    IDF = carve([P], F32)
    IDB = carve([P], BF16)
    ONES_D = carve([P], BF16)
    ONES_V = carve([P], BF16)
    G = carve([P], F32)
    GH = carve([P], F32)
    CW = carve([P], F32)
    MASK = carve([H, P], F32)
    DQ = carve([H, P], F32)
    DKH = carve([H], F32)
    EPSC = carve([1], F32)
    PT = carve([2, 2, TT], BF16)
    CS = carve([2, TT], F32)
    XS = Ring([carve([D], F32) for _ in range(2)])
    PS_ST = Ring([carve([DPLE], F32) for _ in range(2)])
    LOADT = carve([P], F32)
    XN = carve([KC, TT], BF16)
    RSTD = Ring([carve([TT], F32) for _ in range(2)])
    SQ = Ring([carve([TT], BF16) for _ in range(6)])
    TMPA = Ring([carve([TT], F32) for _ in range(3)])
    TMPB = Ring([carve([TT], F32) for _ in range(3)])
    ST_R = Ring([carve([P], BF16) for _ in range(5)])
    KT_R = Ring([carve([2 * P], BF16) for _ in range(5)])
    RSG_R = Ring([carve([P], F32) for _ in range(3)])
    WBYTES = 40 * 1024
    w_base = cursor[0]
    cursor[0] += WBYTES
    scr0 = cursor[0]
    Hh = carve_at(scr0, [FC, TT], BF16)
    Y = carve_at(scr0 + 22 * 1024, [KC, TT], F32)
    U = carve_at(scr0, [KC, TT + 2], F32)
    BV = carve_at(scr0 + 38 * 1024, [KC, TT], BF16)
    YR = carve_at(scr0, [H * 4, TT], BF16)
    Q = carve_at(scr0 + 16 * 1024, [H * 2, TT], BF16)
    QD = carve_at(scr0 + 24 * 1024, [H * 2, TT], BF16)
    Kb = carve_at(scr0 + 32 * 1024, [H * 2, TT], BF16)
    V = carve_at(scr0 + 40 * 1024, [NCH, H * DV], BF16)
    assert scr0 + 56 * 1024 <= ARENA_BYTES, scr0
    OSLOT = [carve_at(scr0 + i * 4096, [D], F32) for i in range(NCH)]
    ISLOT = [carve_at(scr0 + 16 * 1024 + i * 4096, [D], F32) for i in range(NCH)]
    print("sbuf bytes used", scr0 + 56 * 1024)

    PSB = Buf(psum, "ps", 0, [8, 512], 4)
    PSB16 = Buf(psum.bitcast(BF16), "ps", 0, [8, 1024], 2)
    ps_i = [0]

    def ps_next():
        b = ps_i[0] % 6
        ps_i[0] += 1
        return b

    ss_i = [0]

    def ss_next():
        b = 6 + ss_i[0] % 2
        ss_i[0] += 1
        return b

    w_cur = [0]

    def walloc(shape):
        nbytes = int(np.prod(shape)) * 2
        nb = (nbytes + BLK - 1) // BLK * BLK
        assert nb <= WBYTES
        if w_cur[0] + nb > WBYTES:
            w_cur[0] = 0
        off = w_base + w_cur[0]
        w_cur[0] += nb
        return carve_at(off, shape, BF16)

    def mm(out, lhsT, rhs, start, stop):
        return pg.add("pe", lambda t: t.matmul(out.ap, lhsT.ap, rhs.ap, start=start, stop=stop),
                      reads=[lhsT, rhs], writes=[out])

    def warm(n):
        for _ in range(n):
            pg.add("pe", lambda t: t.matmul(PSB[5].ap, ONES_D.all().ap, XN[0].ap, start=True, stop=True),
                   reads=[ONES_D.all()], writes=[])

    def tr(out, in_, ident):
        return pg.add("pe", lambda t: t.transpose(out.ap, in_.ap, ident.ap), reads=[in_, ident], writes=[out])

    def act(out, in_, func, scale=1.0, bias=None, extra_reads=()):
        rd = [in_] + list(extra_reads)
        if bias is not None:
            rd.append(bias)
        sc = scale.ap if isinstance(scale, View) else scale
        if isinstance(scale, View):
            rd.append(scale)
        if bias is not None:
            f = lambda a: a.activation(out.ap, in_.ap, func, bias=bias.ap, scale=sc)
        else:
            f = lambda a: a.activation(out.ap, in_.ap, func, scale=sc)
        return pg.add("act", f, reads=rd, writes=[out])

    def pl(e):
        if e == "pool" and cur_tile[0] == 0:
            return "dve"
        return e

    def tt(out, in0, in1, op, in1_ap=None, eng="dve"):
        a1 = in1.ap if in1_ap is None else in1_ap
        return pg.add(pl(eng), lambda v: v.tensor_tensor(out=out.ap, in0=in0.ap, in1=a1, op=op),
                      reads=[in0, in1], writes=[out])

    def stt(out, in0, scalar, in1, op0, op1, eng="dve"):
        if isinstance(scalar, View):
            return pg.add(pl(eng), lambda v: v.scalar_tensor_tensor(out=out.ap, in0=in0.ap, scalar=scalar.ap, in1=in1.ap,
                                                                    op0=op0, op1=op1),
                          reads=[in0, scalar, in1], writes=[out])
        return pg.add(pl(eng), lambda v: v.scalar_tensor_tensor(out=out.ap, in0=in0.ap, scalar=scalar, in1=in1.ap,
                                                                op0=op0, op1=op1),
                      reads=[in0, in1], writes=[out])

    def ts(out, in0, scalar, op, eng="dve"):
        if isinstance(scalar, View):
            return pg.add(pl(eng), lambda v: v.tensor_scalar(out=out.ap, in0=in0.ap, scalar1=scalar.ap, scalar2=None, op0=op),
                          reads=[in0, scalar], writes=[out])
        return pg.add(pl(eng), lambda v: v.tensor_scalar(out=out.ap, in0=in0.ap, scalar1=scalar, scalar2=None, op0=op),
                      reads=[in0], writes=[out])

    def vcopy(out, in_):
        return pg.add("dve", lambda v: v.tensor_copy(out=out.ap, in_=in_.ap), reads=[in_], writes=[out])

    def vmemset(out, val):
        return pg.add("dve", lambda v: v.memset(out.ap, val), writes=[out])

    def vrecip(out, in_):
        return pg.add("dve", lambda v: v.reciprocal(out=out.ap, in_=in_.ap), reads=[in_], writes=[out])

    def dma_in(eng, out, src_ap):
        if eng == "pool":
            return pg.add("pool", lambda g: g.dma_start(out=out.ap, in_=src_ap), writes=[out], dma=True)
        return pg.add("sp", lambda s: s.dma_start(out=out.ap, in_=src_ap), writes=[out], dma=True)

    def dma_out(dst_ap, src):
        return pg.add("act", lambda s: s.dma_start(out=dst_ap, in_=src.ap), reads=[src], dma=True)

    def wload(src_ap, shape, key):
        n = P * int(np.prod(shape))
        if key not in wreg:
            off = wscr_cur[0]
            wscr_cur[0] += n
            assert wscr_cur[0] <= WSCR_ELEMS
            if len(shape) == 2:
                dst = wscr[off:off + n].rearrange("(p k c) -> p k c", p=P, k=shape[0])
            else:
                dst = wscr[off:off + n].rearrange("(p c) -> p c", p=P)
            cops = []
            nk = shape[0]
            step = 8 if nk > 8 else nk
            for k0 in range(0, nk, step):
                k1 = min(nk, k0 + step)
                d_ = dst[:, k0:k1]
                s_ = src_ap[:, k0:k1]
                cops.append(pg.add("pool", lambda g, d_=d_, s_=s_: g.dma_start(out=d_, in_=s_), dma=True))
            wreg[key] = (off, cops, dst)
        off, cops, dst = wreg[key]
        wb = walloc(shape)
        op = dma_in("sp", wb.all(), dst)
        op.deps.update(cops)
        return wb

    def wview(w, l, c0, c1):
        return w[l].rearrange("(k p) n -> p k n", p=P)[:, :, c0:c1]

    vmemset(S.all(), 0.0)
    vmemset(Sb.all(), 0.0)
    vmemset(HALO.all(), 0.0)
    vmemset(EPSC.all(), EPS)
    vmemset(LOADT.all(), 0.0)
    dma_in("sp", IDF.all(), c_ident)
    dma_in("pool", IDB.all(), c_ident)
    dma_in("sp", MASK.all(), c_mask)
    dma_in("sp", DQ.all(), c_dq)
    dma_in("sp", DKH.all(), c_dk)
    t_ones = TMPA.next()
    vmemset(t_ones[0:P], 1.0 / D)
    vcopy(ONES_D.all(), t_ones[0:P])
    t_ones = TMPA.next()
    vmemset(t_ones[0:P], 1.0 / DV)
    vcopy(ONES_V.all(), t_ones[0:P])
    xs0 = XS.next()
    dma_in("sp", xs0[0:P], ng_d.rearrange("l n (c p) -> (l n c) p", p=P))
    b = ps_next()
    tr(PSB[b, 0:P], xs0[0:P], IDF.all())
    vcopy(G.all(), PSB[b, 0:P])
    ts(GH.all(), G.all(), 0.5, ALU.mult)
    dma_in("sp", Buf(LOADT.ap[0:24], "sb", LOADT.off, [P], 4).all(), w_cw[0].rearrange("k (c p) -> (k c) p", p=P))
    b = ps_next()
    tr(PSB[b, 0:P], LOADT.all(), IDF.all())
    vcopy(CW.all(), PSB[b, 0:P])

    def gcol(Gb, l, n, c):
        j = (l * 8 + n) * 8 + c
        return Gb[j:j + 1]

    def rstd_from(ssb):
        r = RSTD.next()
        act(r.all(), PSB[ssb], AF.Sqrt, bias=EPSC.all())
        vrecip(r.all(), r.all())
        return r

    def prenorm(l, n):
        pg.tag = pg.tag.split('.')[0] + '.pre'
        ssb = ss_next()
        for c in range(KC):
            sq = SQ.next()
            act(sq.all(), X[c], AF.Square)
            mm(PSB[ssb], ONES_D.all(), sq.all(), c == 0, c == KC - 1)
        r = rstd_from(ssb)
        for c in range(KC):
            if c >= 5 and cur_tile[0] > 0:
                tmp = TMPB.next()
                tt(tmp.all(), X[c], r.all(), ALU.mult, eng="pool")
                act(XN[c], tmp.all(), AF.Copy, scale=gcol(G, l, n, c))
            else:
                stt(XN[c], X[c], gcol(G, l, n, c), r.all(), ALU.mult, ALU.mult)

    class PostNorm:
        def __init__(self, l, n, half):
            self.ssb = ss_next()
            self.n = 0
            self.pending = None
            self.l, self.nn = l, n
            self.Gb = GH if half else G
            self.scaled = True

        def _flush(self):
            if self.pending is not None:
                sq = self.pending
                self.pending = None
                mm(PSB[self.ssb], ONES_D.all(), sq.all(), self.n == 0, self.n == KC - 1)
                self.n += 1

        def add_from_psum(self, j, psv):
            self._flush()
            act(Y[j], psv, AF.Copy, scale=gcol(self.Gb, self.l, self.nn, j))
            sq = SQ.next()
            act(sq.all(), psv, AF.Square)
            self.pending = sq

        def add_from_y(self, j):
            self.scaled = False
            self._flush()
            sq = SQ.next()
            act(sq.all(), Y[j], AF.Square)
            self.pending = sq

        def finish(self):
            self._flush()
            pg.tag = pg.tag.split('.')[0] + '.post'
            assert self.n == KC
            r = rstd_from(self.ssb)
            for j in range(KC):
                t = TMPA.next()
                if self.scaled:
                    e = "pool" if j in (1, 4, 7) else "dve"
                    tt(t.all(), Y[j], r.all(), ALU.mult, eng=e)
                    tt(X[j], t.all(), X[j], ALU.add, eng=e)
                else:
                    tt(t.all(), Y[j], r.all(), ALU.mult, eng="pool")
                    stt(X[j], t.all(), gcol(self.Gb, self.l, self.nn, j), X[j], ALU.mult, ALU.add)

    def ffn(l, wg, wu, wd, n_pre, n_post, tag):
        pg.tag = tag
        prenorm(l, n_pre)
        pg.tag = tag + '.gu'
        tiles_ = {}

        def gu_tile(f2):
            if f2 not in tiles_:
                tiles_[f2] = (wload(wview(wg, l, f2 * 256, (f2 + 1) * 256), [KC, 256], (tag, 'g', l, f2)),
                              wload(wview(wu, l, f2 * 256, (f2 + 1) * 256), [KC, 256], (tag, 'u', l, f2)))
            return tiles_[f2]

        def gu_evac(f, bg, bu):
            sg = TMPB.next()
            act(sg.all(), PSB[bg], AF.Silu)
            tt(Hh[f], sg.all(), PSB[bu], ALU.mult)

        head = [(0, 0), (0, 1), (1, 0)]
        hb_ = []
        for (f2, hf) in head:
            gu_tile(f2)
            hb_.append((ps_next(), ps_next()))
        for k in range(KC):
            for (f2, hf), (bg, bu) in zip(head, hb_):
                g_t, u_t = tiles_[f2]
                mm(PSB[bg], g_t[k, hf * P:(hf + 1) * P], XN[k], k == 0, k == KC - 1)
                mm(PSB[bu], u_t[k, hf * P:(hf + 1) * P], XN[k], k == 0, k == KC - 1)
        for (f2, hf), (bg, bu) in zip(head, hb_):
            gu_evac(2 * f2 + hf, bg, bu)
        for f2 in range(FC // 2):
            for hf in range(2):
                if (f2, hf) in head:
                    continue
                g_t, u_t = gu_tile(f2)
                bg, bu = ps_next(), ps_next()
                for k in range(KC):
                    mm(PSB[bg], g_t[k, hf * P:(hf + 1) * P], XN[k], k == 0, k == KC - 1)
                for k in range(KC):
                    mm(PSB[bu], u_t[k, hf * P:(hf + 1) * P], XN[k], k == 0, k == KC - 1)
                gu_evac(2 * f2 + hf, bg, bu)
        pg.tag = tag + '.dn'
        pn = PostNorm(l, n_post, True)
        for j2 in range(KC // 2):
            d_t = wload(wd[l].rearrange("(k p) n -> p k n", p=P)[:, :, j2 * 256:(j2 + 1) * 256], [FC, 256], (tag, 'd', l, j2))
            for hf in range(2):
                j = 2 * j2 + hf
                by = ps_next()
                for f in range(FC):
                    mm(PSB[by], d_t[f, hf * P:(hf + 1) * P], Hh[f], f == 0, f == FC - 1)
                pn.add_from_psum(j, PSB[by])
        pn.finish()

    def conv_mixer(l):
        pg.tag = 'conv'
        prenorm(l, MIX_PRE)
        pg.tag = 'conv.in'
        for m2 in range(KC // 2):
            wb_ = wload(wview(w_cin, 0, m2 * 256, (m2 + 1) * 256), [KC, 256], ('cb', m2))
            wc_ = wload(wview(w_cin, 0, D + m2 * 256, D + (m2 + 1) * 256), [KC, 256], ('cc', m2))
            wh_ = wload(wview(w_cin, 0, 2 * D + m2 * 256, 2 * D + (m2 + 1) * 256), [KC, 256], ('ch', m2))
            for hf in range(2):
                m = 2 * m2 + hf
                bb, bc, bh = ps_next(), ps_next(), ps_next()
                for k in range(KC):
                    mm(PSB[bc], wc_[k, hf * P:(hf + 1) * P], XN[k], k == 0, k == KC - 1)
                for k in range(KC):
                    mm(PSB[bh], wh_[k, hf * P:(hf + 1) * P], XN[k], k == 0, k == KC - 1)
                for k in range(KC):
                    mm(PSB[bb], wb_[k, hf * P:(hf + 1) * P], XN[k], k == 0, k == KC - 1)
                cs = TMPB.next()
                act(cs.all(), PSB[bc], AF.Copy)
                bsb = TMPB.next()
                act(bsb.all(), PSB[bb], AF.Copy)
                vcopy(U[m, 0:2], HALO[m])
                tt(U[m, 2:TT + 2], cs.all(), PSB[bh], ALU.mult)
                v = TMPA.next()
                act(v.all(), U[m, 2:TT + 2], AF.Copy, scale=CW[16 + m:17 + m])
                stt(v.all(), U[m, 1:TT + 1], CW[8 + m:9 + m], v.all(), ALU.mult, ALU.add)
                stt(v.all(), U[m, 0:TT], CW[m:m + 1], v.all(), ALU.mult, ALU.add)
                tt(BV[m], v.all(), bsb.all(), ALU.mult, eng="pool")
                vcopy(HALO[m], U[m, TT:TT + 2])
        pg.tag = 'conv.out'
        pn = PostNorm(l, MIX_POST, False)
        for j2 in range(KC // 2):
            o_t = wload(wview(w_cout, 0, j2 * 256, (j2 + 1) * 256), [KC, 256], ('co', j2))
            for hf in range(2):
                j = 2 * j2 + hf
                by = ps_next()
                for m in range(KC):
                    mm(PSB[by], o_t[m, hf * P:(hf + 1) * P], BV[m], m == 0, m == KC - 1)
                pn.add_from_psum(j, PSB[by])
        pn.finish()

    GAMMA = [1.0 - 2.0 ** (-5 - h) for h in range(H)]
    GAMMA_C = [float(np.float32(g) ** np.float32(CH)) for g in GAMMA]

    def retention(l):
        pg.tag = 'ret'
        prenorm(l, MIX_PRE)
        pg.tag = 'ret.qk'
        qoff, koff, voff, goff = 0, H * DK, 2 * H * DK, 2 * H * DK + H * DV
        for h in range(H):
            for which in range(2):
                base = (qoff if which == 0 else koff) + h * DK
                w_t = wload(wview(w_rin, 0, base, base + DK), [KC, DK], ('rqk', h, which))
                b1, b2 = ps_next(), ps_next()
                for k in range(KC):
                    mm(PSB[b1], w_t[k, 0:P], XN[k], k == 0, k == KC - 1)
                for k in range(KC):
                    mm(PSB[b2], w_t[k, P:2 * P], XN[k], k == 0, k == KC - 1)
                dst = Q if which == 0 else Kb
                a = TMPA.next()
                bt = TMPB.next()
                tt(a.all(), PSB[b1], CS[0], ALU.mult)
                tt(bt.all(), PSB[b2], CS[1], ALU.mult)
                tt(dst[2 * h], a.all(), bt.all(), ALU.subtract, eng="pool")
                a = TMPA.next()
                bt = TMPB.next()
                tt(a.all(), PSB[b2], CS[0], ALU.mult)
                tt(bt.all(), PSB[b1], CS[1], ALU.mult)
                tt(dst[2 * h + 1], a.all(), bt.all(), ALU.add, eng="pool")
                if which == 0:
                    dq_b = DQ[h].ap.unsqueeze(1).to_broadcast([P, NCH, P])
                    for hh in range(2):
                        qd3 = QD[2 * h + hh].ap.rearrange("p (a b) -> p a b", a=NCH)
                        q3 = Q[2 * h + hh].ap.rearrange("p (a b) -> p a b", a=NCH)
                        pg.add(pl("pool"), lambda v, qd3=qd3, q3=q3, dq_b=dq_b: v.tensor_tensor(out=qd3, in0=q3, in1=dq_b, op=ALU.mult),
                               reads=[Q[2 * h + hh], DQ[h]], writes=[QD[2 * h + hh]])
        pg.tag = 'ret.v'
        for vb in range(H):
            w_t = wload(wview(w_rin, 0, voff + vb * DV, voff + (vb + 1) * DV), [KC, DV], ('rv', vb))
            for n in range(NCH):
                bv_ = ps_next()
                for k in range(KC):
                    mm(PSB[bv_], XN[k, n * P:(n + 1) * P], w_t[k], k == 0, k == KC - 1)
                act(V[n, vb * DV:(vb + 1) * DV], PSB[bv_], AF.Copy)
        pg.tag = 'ret.core'
        for n in range(NCH):
            tsl = slice(n * P, (n + 1) * P)
            sts, kts, bos, sqs = [], [], [], []
            for h in range(H):
                bs = ps_next()
                for dc in range(2):
                    mm(PSB[bs, 0:P], Kb[2 * h + dc, tsl], Q[2 * h + dc, tsl], dc == 0, dc == 1)
                for dc in range(2):
                    tr(PSB16[bs, 512 + dc * P:512 + (dc + 1) * P], Kb[2 * h + dc, tsl], IDB.all())
                st_ = ST_R.next()
                s_ap = PSB[bs, 0:P].ap
                pg.add("dve", lambda v, o=st_.all().ap, i0=s_ap, i1=MASK[h].ap: v.tensor_tensor(out=o, in0=i0, in1=i1, op=ALU.mult),
                       reads=[PSB[bs], MASK[h]], writes=[st_.all()])
                kt = KT_R.next()
                k_ap = PSB16[bs, 512:512 + 2 * P].ap
                pg.add("act", lambda a_, o=kt.all().ap, i0=k_ap, sc=DKH[h:h + 1].ap: a_.activation(o, i0, AF.Copy, scale=sc),
                       reads=[PSB[bs], DKH[h:h + 1]], writes=[kt.all()])
                sts.append(st_)
                kts.append(kt)
            for h in range(H):
                bo = ps_next()
                for ec in range(4):
                    osl = slice(ec * P, (ec + 1) * P)
                    mm(PSB[bo, osl], V[n, h * DV + ec * P:h * DV + (ec + 1) * P], sts[h].all(), True, False)
                    for dc in range(2):
                        mm(PSB[bo, osl], Sb[2 * h + dc, ec * P:(ec + 1) * P], QD[2 * h + dc, tsl], False, dc == 1)
                sq = SQ.next()
                act(sq.all(), PSB[bo], AF.Square)
                bos.append(bo)
                sqs.append(sq)
            for h in range(H):
                bo, sq = bos[h], sqs[h]
                bgn = ss_next()
                for ec in range(4):
                    mm(PSB[bgn, 0:P], ONES_V.all(), sq[ec * P:(ec + 1) * P], ec == 0, ec == 3)
                rs = RSG_R.next()
                act(rs.all(), PSB[bgn, 0:P], AF.Sqrt, bias=EPSC.all())
                vrecip(rs.all(), rs.all())
                rs_b = rs.all().ap.unsqueeze(1).to_broadcast([P, 4, P])
                o3 = PSB[bo].ap.rearrange("p (a b) -> p a b", a=4)
                yr3 = YR[4 * h:4 * h + 4, tsl]
                pg.add("dve", lambda v, yr3=yr3, o3=o3, rs_b=rs_b: v.tensor_tensor(out=yr3.ap, in0=o3, in1=rs_b, op=ALU.mult),
                       reads=[PSB[bo], rs.all()], writes=[yr3])
            for h in range(H):
                for dc in range(2):
                    bd = ps_next()
                    mm(PSB[bd], kts[h][dc * P:(dc + 1) * P], V[n, h * DV:(h + 1) * DV], True, True)
                    stt(S[2 * h + dc], S[2 * h + dc], GAMMA_C[h], PSB[bd], ALU.mult, ALU.add)
                    if cur_tile[0] > 0:
                        pg.add("pool", lambda g, o=Sb[2 * h + dc].ap, i=S[2 * h + dc].ap: g.tensor_copy(out=o, in_=i),
                               reads=[S[2 * h + dc]], writes=[Sb[2 * h + dc]])
                    else:
                        act(Sb[2 * h + dc], S[2 * h + dc], AF.Copy)
        pg.tag = 'ret.g'
        for gb in range(H * DV // 256):
            w_t = wload(wview(w_rin, 0, goff + gb * 256, goff + (gb + 1) * 256), [KC, 256], ('rg', gb))
            for hf in range(2):
                c = 2 * gb + hf
                bg = ps_next()
                for k in range(KC):
                    mm(PSB[bg], w_t[k, hf * P:(hf + 1) * P], XN[k], k == 0, k == KC - 1)
                sgt = TMPB.next()
                act(sgt.all(), PSB[bg], AF.Silu)
                tt(YR[c], YR[c], sgt.all(), ALU.mult)
        pg.tag = 'ret.out'
        pn = PostNorm(l, MIX_POST, False)
        for j2 in range(KC // 2):
            o_t = wload(wview(w_rout, 0, j2 * 256, (j2 + 1) * 256), [H * 4, 256], ('ro', j2))
            for hf in range(2):
                j = 2 * j2 + hf
                by = ps_next()
                for c in range(H * 4):
                    mm(PSB[by], o_t[c, hf * P:(hf + 1) * P], YR[c], c == 0, c == H * 4 - 1)
                pn.add_from_psum(j, PSB[by])
        pn.finish()

    def ple(l):
        pg.tag = 'ple'
        prenorm(l, PLE_PRE)
        pg.tag = 'ple.main'
        pn = PostNorm(l, PLE_POST, False)
        for j2 in range(KC // 2):
            g_t = wload(wview(w_pg, l, j2 * 256, (j2 + 1) * 256), [KC, 256], ('pg', l, j2))
            p_t = wload(wview(w_pp, l, j2 * 256, (j2 + 1) * 256), [2, 256], ('pp', l, j2))
            for hf in range(2):
                j = 2 * j2 + hf
                bg, be = ps_next(), ps_next()
                for k in range(KC):
                    mm(PSB[bg], g_t[k, hf * P:(hf + 1) * P], XN[k], k == 0, k == KC - 1)
                for k in range(2):
                    mm(PSB[be], p_t[k, hf * P:(hf + 1) * P], PT[l, k], k == 0, k == 1)
                sg = TMPB.next()
                act(sg.all(), PSB[bg], AF.Sigmoid)
                tt(Y[j], sg.all(), PSB[be], ALU.mult)
                pn.add_from_y(j)
        pn.finish()

    out_dmas = []

    def load_tile(ti):
        pg.tag = 'load'
        t0 = ti * TT
        for n in range(NCH):
            dma_in("sp", ISLOT[n].all(), x_d[t0 + n * P:t0 + (n + 1) * P, :])
        for n in range(NCH):
            xs = ISLOT[n]
            for hb in range(2):
                b = ps_next()
                for q in range(4):
                    c = hb * 4 + q
                    tr(PSB[b, q * P:(q + 1) * P], xs[c * P:(c + 1) * P], IDF.all())
                dst = X[hb * 4:hb * 4 + 4, n * P:(n + 1) * P]
                src = PSB[b].ap.rearrange("p (a b) -> p a b", a=4)
                pg.add("act", lambda a, dst=dst, src=src: a.activation(dst.ap, src, AF.Copy),
                       reads=[PSB[b]], writes=[dst])
            for l in layers:
                pst = PS_ST.next()
                dma_in("sp", pst.all(), p_d[l, t0 + n * P:t0 + (n + 1) * P, :])
                b = ps_next()
                for k in range(2):
                    tr(PSB[b, k * P:(k + 1) * P], pst[k * P:(k + 1) * P], IDF.all())
                dst = PT[l, 0:2, n * P:(n + 1) * P]
                src = PSB[b, 0:2 * P].ap.rearrange("p (a b) -> p a b", a=2)
                pg.add("dve", lambda v, dst=dst, src=src: v.tensor_copy(out=dst.ap, in_=src),
                       reads=[PSB[b, 0:2 * P]], writes=[dst])
        if 1 in layers:
            dma_in("sp", CS[0], c_cos[:, t0:t0 + TT])
            dma_in("sp", CS[1], c_sin[:, t0:t0 + TT])

    def store_tile(ti):
        pg.tag = 'store'
        t0 = ti * TT
        for n in range(NCH):
            xs = OSLOT[n]
            for hb in range(2):
                b = ps_next()
                for q in range(4):
                    c = hb * 4 + q
                    tr(PSB[b, q * P:(q + 1) * P], X[c, n * P:(n + 1) * P], IDF.all())
                act(xs[hb * 4 * P:(hb * 4 + 4) * P], PSB[b], AF.Copy)
            out_dmas.append(dma_out(y_d[t0 + n * P:t0 + (n + 1) * P, :], xs.all()))

    for ti in range(ntile):
        cur_tile[0] = ti
        load_tile(ti)
        for l in layers:
            ffn(l, w_f1g, w_f1u, w_f1d, FFN1_PRE, FFN1_POST, 'f1')
            if l % 2 == 0:
                conv_mixer(l)
            else:
                retention(l)
            ffn(l, w_f2g, w_f2u, w_f2d, FFN2_PRE, FFN2_POST, 'f2')
            ple(l)
        store_tile(ti)
    fin = pg.add("sp", None)
    fin.deps.update(out_dmas)

    pg.finalize()

    sem_cms = []
    sems = {}

    def mksem(key, name):
        cm = nc.semaphore(name)
        sem_cms.append(cm)
        sems[key] = cm.__enter__()

    for e in ("pe", "act", "dve", "pool", "sp"):
        mksem((e, "c"), "c_" + e)
    for e in ("pool", "sp", "act"):
        for s_ in range(Prog.NDMA_SEM):
            mksem((e, s_), "d_%s_%d" % (e, s_))

    with nc.Block() as block:
        @block.tensor
        def _(t):
            pg.emit_engine("pe", t, sems)

        @block.scalar
        def _(a):
            pg.emit_engine("act", a, sems)

        @block.vector
        def _(v):
            pg.emit_engine("dve", v, sems)

        @block.gpsimd
        def _(g):
            pg.emit_engine("pool", g, sems)

        @block.sync
        def _(s):
            pg.emit_engine("sp", s, sems)

    for cm in reversed(sem_cms):
        cm.__exit__(None, None, None)
    psum_cm.__exit__(None, None, None)
    arena_cm.__exit__(None, None, None)
    nstat = {e: len(pg.ops[e]) for e in Prog.ENGS}
    nstat["_pe_tags"] = [op.tag for op in pg.ops["pe"]]
    return nc, nstat


def make_consts(T):
    f32 = np.float32
    ident = np.eye(P, dtype=f32)
    gam = np.array([1.0 - 2.0 ** (-5 - h) for h in range(H)], dtype=np.float64)
    idx = np.arange(CH, dtype=np.float64)
    diff = idx[None, :] - idx[:, None]
    mask = np.zeros((P, H, P), dtype=f32)
    for h in range(H):
        m = np.where(diff >= 0, gam[h] ** np.maximum(diff, 0.0), 0.0) * (DK ** -0.5)
        mask[:, h, :] = m.astype(f32)
    dq = np.zeros((P, H, P), dtype=f32)
    for h in range(H):
        row = gam[h] ** (idx + 1.0)
        dq[:, h, :] = row.astype(f32)[None, :]
    dk = np.zeros((P, H), dtype=f32)
    for h in range(H):
        dk[:, h] = (gam[h] ** (CH - 1.0 - idx) * (DK ** -0.5)).astype(f32)
    half = DK // 2
    inv_freq = (1.0 / (np.float32(10000.0) ** np.linspace(0.0, 1.0, half, dtype=f32))).astype(f32)
    pos = np.arange(T, dtype=f32)
    ang = (inv_freq[:, None] * pos[None, :]).astype(f32)
    return {
        "c_ident": ident, "c_mask": mask, "c_dq": dq, "c_dk": dk,
        "c_cos": np.cos(ang).astype(f32), "c_sin": np.sin(ang).astype(f32),
    }


_WNAMES = ["norm_g", "ffn1_w_gate", "ffn1_w_up", "ffn1_w_down", "ffn2_w_gate", "ffn2_w_up", "ffn2_w_down",
           "conv_w_in", "conv_w", "conv_w_out", "ret_w_in", "ret_w_out", "ple_w_proj", "ple_w_gate"]


def run(inputs, T, n_cores, layers=(0, 1), trace=False):
    nc, _ = build(T, layers)
    consts = make_consts(T)
    shared = {k: np.ascontiguousarray(np.asarray(inputs[k], dtype=np.float32)) for k in _WNAMES}
    shared.update(consts)
    x = np.asarray(inputs["x"], dtype=np.float32)
    p = np.asarray(inputs["p"], dtype=np.float32)
    in_maps = []
    for c in range(n_cores):
        m = dict(shared)
        m["x"] = np.ascontiguousarray(x[c])
        m["p"] = np.ascontiguousarray(p[:, c])
        in_maps.append(m)
    res = run_bass_kernel_spmd(nc, in_maps, core_ids=list(range(n_cores)), trace=trace)
    out = np.stack([np.asarray(r["y"]) for r in res.results], axis=0)
    return out, res


def kernel(**inputs):
    out, _ = run(inputs, SEQ, N_CORES)
    return out.astype(np.float32)
```

```python
import numpy as np
import concourse.bass as bass
import concourse.mybir as mybir
from concourse.bass_utils import run_bass_kernel_spmd

F32 = mybir.dt.float32
BF16 = mybir.dt.bfloat16
AF = mybir.ActivationFunctionType
ALU = mybir.AluOpType

P = 128
D = 1024
KC = D // P
DFF = 2816
FC = DFF // P
DPLE = 256
TT = 512
NCH = TT // P
EPS = 1e-6
H = 4
DK = 256
DV = 512
CH = 128
N_CORES = 8
SEQ = 4096
BLK = 256

FFN1_PRE, FFN1_POST, MIX_PRE, MIX_POST, FFN2_PRE, FFN2_POST, PLE_PRE, PLE_POST = range(8)


class View:
    __slots__ = ("ap", "blocks")

    def __init__(self, ap, blocks):
        self.ap = ap
        self.blocks = blocks


class Buf:
    def __init__(self, ap, space, off, shape, esz):
        self.ap, self.space, self.off, self.shape, self.esz = ap, space, off, tuple(shape), esz
        st, acc = [], 1
        for n in reversed(self.shape):
            st.append(acc)
            acc *= n
        self.strides = tuple(reversed(st))
        self.nbytes = acc * esz

    def __getitem__(self, idx):
        if not isinstance(idx, tuple):
            idx = (idx,)
        idx = idx + (slice(None),) * (len(self.shape) - len(idx))
        rng = []
        for i, n in zip(idx, self.shape):
            if isinstance(i, int):
                rng.append((i, i + 1))
            else:
                a, b, s = i.indices(n)
                assert s == 1
                rng.append((a, b))
        L = 1
        d = len(rng) - 1
        while d >= 0:
            a, b = rng[d]
            if a == 0 and b == self.shape[d]:
                L *= self.shape[d]
                d -= 1
                continue
            break
        starts = [0]
        if d >= 0:
            a, b = rng[d]
            L *= (b - a)
            starts = [a * self.strides[d]]
            for dd in range(d - 1, -1, -1):
                a, b = rng[dd]
                starts = [s0 + i * self.strides[dd] for i in range(a, b) for s0 in starts]
        blocks = set()
        for s0 in starts:
            lo = (self.off + s0 * self.esz) // BLK
            hi = (self.off + (s0 + L) * self.esz - 1) // BLK
            for b in range(lo, hi + 1):
                blocks.add((self.space, b))
        return View(self.ap[(slice(None),) + idx], blocks)

    def all(self):
        return self[tuple(slice(None) for _ in self.shape)]


class Op:
    __slots__ = ("eng", "fn", "deps", "sig", "sem", "val", "inc", "is_dma", "tag")

    def __init__(self, eng, fn, is_dma):
        self.eng, self.fn, self.is_dma = eng, fn, is_dma
        self.deps = set()
        self.sig = is_dma
        self.sem = None
        self.val = 0
        self.inc = 16 if is_dma else 1


class Prog:
    ENGS = ("pe", "act", "dve", "pool", "sp")
    NDMA_SEM = 12
    NSEM = {"pool": 3}

    def __init__(self):
        self.ops = {e: [] for e in self.ENGS}
        self.state = {}
        self.dma_count = {e: 0 for e in self.ENGS}
        self.dma_last = {}
        self.tag = ""

    def add(self, eng, fn, reads=(), writes=(), dma=False):
        op = Op(eng, fn, dma)
        op.tag = self.tag
        deps = op.deps
        st = self.state
        for v in reads:
            for b in v.blocks:
                e = st.get(b)
                if e is not None and e[0] is not None:
                    deps.add(e[0])
        for v in writes:
            for b in v.blocks:
                e = st.get(b)
                if e is not None:
                    if e[0] is not None:
                        deps.add(e[0])
                    deps.update(e[1])
        for v in reads:
            for b in v.blocks:
                e = st.get(b)
                if e is None:
                    st[b] = [None, [op]]
                else:
                    e[1].append(op)
        for v in writes:
            for b in v.blocks:
                st[b] = [op, []]
        deps.discard(op)
        if dma:
            n = self.dma_count[eng]
            self.dma_count[eng] = n + 1
            nsem = self.NSEM.get(eng, self.NDMA_SEM)
            slot = n % nsem
            prev = self.dma_last.get((eng, slot))
            if prev is not None:
                deps.add(prev)
            self.dma_last[(eng, slot)] = op
            op.sem = (eng, slot)
            op.val = 16 * (n // nsem + 1)
        self.ops[eng].append(op)
        return op

    def finalize(self):
        for e in self.ENGS:
            for op in self.ops[e]:
                for d in op.deps:
                    if d.is_dma:
                        continue
                    if d.eng == "pe" and e == "pe":
                        continue
                    d.sig = True
        for e in self.ENGS:
            cnt = 0
            for op in self.ops[e]:
                if op.is_dma:
                    continue
                if op.sig:
                    cnt += 1
                    op.sem = (e, "c")
                    op.val = cnt

    def emit_engine(self, ename, eng, sems):
        seen = {}
        for op in self.ops[ename]:
            need = {}
            for d in op.deps:
                if (not d.is_dma) and d.eng == "pe" and ename == "pe":
                    continue
                if d.val > need.get(d.sem, 0):
                    need[d.sem] = d.val
            for k, v in need.items():
                if seen.get(k, 0) >= v:
                    continue
                eng.wait_ge(sems[k], v)
                seen[k] = v
            if op.fn is not None:
                ins = op.fn(eng)
                if op.sig:
                    ins.then_inc(sems[op.sem], op.inc)


class Ring:
    def __init__(self, bufs):
        self.bufs = bufs
        self.i = 0

    def next(self):
        b = self.bufs[self.i % len(self.bufs)]
        self.i += 1
        return b


def build(T=SEQ, layers=(0, 1)):
    nc = bass.Bass("TRN2", target_bir_lowering=False)
    ntile = T // TT
    pg = Prog()

    def din(name, shape):
        return nc.dram_tensor(name, list(shape), F32, kind="ExternalInput").ap()

    x_d = din("x", [T, D])
    p_d = din("p", [2, T, DPLE])
    ng_d = din("norm_g", [2, 8, D])
    w_f1g = din("ffn1_w_gate", [2, D, DFF])
    w_f1u = din("ffn1_w_up", [2, D, DFF])
    w_f1d = din("ffn1_w_down", [2, DFF, D])
    w_f2g = din("ffn2_w_gate", [2, D, DFF])
    w_f2u = din("ffn2_w_up", [2, D, DFF])
    w_f2d = din("ffn2_w_down", [2, DFF, D])
    w_cin = din("conv_w_in", [1, D, 3 * D])
    w_cw = din("conv_w", [1, 3, D])
    w_cout = din("conv_w_out", [1, D, D])
    w_rin = din("ret_w_in", [1, D, 2 * H * DK + 2 * H * DV])
    w_rout = din("ret_w_out", [1, H * DV, D])
    w_pp = din("ple_w_proj", [2, DPLE, D])
    w_pg = din("ple_w_gate", [2, D, D])
    c_ident = din("c_ident", [P, P])
    c_mask = din("c_mask", [P, H, P])
    c_dq = din("c_dq", [P, H, P])
    c_dk = din("c_dk", [P, H])
    c_cos = din("c_cos", [P, T])
    c_sin = din("c_sin", [P, T])
    y_d = nc.dram_tensor("y", [T, D], F32, kind="ExternalOutput").ap()
    WSCR_ELEMS = 2 * (2 * 3 * D * DFF) + D * 3 * D + D * D + D * (2 * H * DK + 2 * H * DV) + H * DV * D + 2 * (DPLE * D + D * D)
    wscr = nc.dram_tensor("wscr", [WSCR_ELEMS], BF16, kind="Internal").ap()
    wreg = {}
    wscr_cur = [0]
    cur_tile = [0]

    ARENA_BYTES = 206 * 1024
    arena_cm = nc.sbuf_tensor("arena", [P, ARENA_BYTES // 4], F32)
    psum_cm = nc.psum_tensor("ps", [P, 8, 512], F32)
    arena = arena_cm.__enter__()
    psum = psum_cm.__enter__()

    cursor = [0]

    def carve_at(off, shape, dt):
        esz = 4 if dt == F32 else 2
        n = int(np.prod(shape))
        nbytes = n * esz
        assert off % 4 == 0
        ap = arena[:, off // 4:(off + nbytes + 3) // 4]
        if dt == BF16:
            ap = ap.bitcast(BF16)
        if len(shape) == 2:
            ap = ap.rearrange("p (a b) -> p a b", a=shape[0])
        elif len(shape) == 3:
            ap = ap.rearrange("p (a b c) -> p a b c", a=shape[0], b=shape[1])
        return Buf(ap, "sb", off, shape, esz)

    def carve(shape, dt):
        esz = 4 if dt == F32 else 2
        nbytes = int(np.prod(shape)) * esz
        off = cursor[0]
        cursor[0] = (off + nbytes + BLK - 1) // BLK * BLK
        assert cursor[0] <= ARENA_BYTES, ("arena overflow", cursor[0])
        return carve_at(off, shape, dt)

    X = carve([KC, TT], F32)
    S = carve([H * 2, DV], F32)
    Sb = carve([H * 2, DV], BF16)
    HALO = carve([KC, 2], F32)
    IDF = carve([P], F32)
    IDB = carve([P], BF16)
    ONES_D = carve([P], BF16)
    ONES_V = carve([P], BF16)
    G = carve([P], F32)
    GH = carve([P], F32)
    CW = carve([P], F32)
    MASK = carve([H, P], F32)
    DQ = carve([H, P], F32)
    DKH = carve([H], F32)
    EPSC = carve([1], F32)
    PT = carve([2, 2, TT], BF16)
    CS = carve([2, TT], F32)
    XS = Ring([carve([D], F32) for _ in range(2)])
    PS_ST = Ring([carve([DPLE], F32) for _ in range(2)])
    LOADT = carve([P], F32)
    XN = carve([KC, TT], BF16)
    RSTD = Ring([carve([TT], F32) for _ in range(2)])
    SQ = Ring([carve([TT], BF16) for _ in range(6)])
    TMPA = Ring([carve([TT], F32) for _ in range(3)])
    TMPB = Ring([carve([TT], F32) for _ in range(3)])
    ST_R = Ring([carve([P], BF16) for _ in range(5)])
    KT_R = Ring([carve([2 * P], BF16) for _ in range(5)])
    RSG_R = Ring([carve([P], F32) for _ in range(3)])
    WBYTES = 40 * 1024
    w_base = cursor[0]
    cursor[0] += WBYTES
    scr0 = cursor[0]
    Hh = carve_at(scr0, [FC, TT], BF16)
    Y = carve_at(scr0 + 22 * 1024, [KC, TT], F32)
    U = carve_at(scr0, [KC, TT + 2], F32)
    BV = carve_at(scr0 + 38 * 1024, [KC, TT], BF16)
    YR = carve_at(scr0, [H * 4, TT], BF16)
    Q = carve_at(scr0 + 16 * 1024, [H * 2, TT], BF16)
    QD = carve_at(scr0 + 24 * 1024, [H * 2, TT], BF16)
    Kb = carve_at(scr0 + 32 * 1024, [H * 2, TT], BF16)
    V = carve_at(scr0 + 40 * 1024, [NCH, H * DV], BF16)
    assert scr0 + 56 * 1024 <= ARENA_BYTES, scr0
    OSLOT = [carve_at(scr0 + i * 4096, [D], F32) for i in range(NCH)]
    ISLOT = [carve_at(scr0 + 16 * 1024 + i * 4096, [D], F32) for i in range(NCH)]
    print("sbuf bytes used", scr0 + 56 * 1024)

    PSB = Buf(psum, "ps", 0, [8, 512], 4)
    PSB16 = Buf(psum.bitcast(BF16), "ps", 0, [8, 1024], 2)
    ps_i = [0]

    def ps_next():
        b = ps_i[0] % 6
        ps_i[0] += 1
        return b

    ss_i = [0]

    def ss_next():
        b = 6 + ss_i[0] % 2
        ss_i[0] += 1
        return b

    w_cur = [0]

    def walloc(shape):
        nbytes = int(np.prod(shape)) * 2
        nb = (nbytes + BLK - 1) // BLK * BLK
        assert nb <= WBYTES
        if w_cur[0] + nb > WBYTES:
            w_cur[0] = 0
        off = w_base + w_cur[0]
        w_cur[0] += nb
        return carve_at(off, shape, BF16)

    def mm(out, lhsT, rhs, start, stop):
        return pg.add("pe", lambda t: t.matmul(out.ap, lhsT.ap, rhs.ap, start=start, stop=stop),
                      reads=[lhsT, rhs], writes=[out])

    def warm(n):
        for _ in range(n):
            pg.add("pe", lambda t: t.matmul(PSB[5].ap, ONES_D.all().ap, XN[0].ap, start=True, stop=True),
                   reads=[ONES_D.all()], writes=[])

    def tr(out, in_, ident):
        return pg.add("pe", lambda t: t.transpose(out.ap, in_.ap, ident.ap), reads=[in_, ident], writes=[out])

    def act(out, in_, func, scale=1.0, bias=None, extra_reads=()):
        rd = [in_] + list(extra_reads)
        if bias is not None:
            rd.append(bias)
        sc = scale.ap if isinstance(scale, View) else scale
        if isinstance(scale, View):
            rd.append(scale)
        if bias is not None:
            f = lambda a: a.activation(out.ap, in_.ap, func, bias=bias.ap, scale=sc)
        else:
            f = lambda a: a.activation(out.ap, in_.ap, func, scale=sc)
        return pg.add("act", f, reads=rd, writes=[out])

    def pl(e):
        if e == "pool" and cur_tile[0] == 0:
            return "dve"
        return e

    def tt(out, in0, in1, op, in1_ap=None, eng="dve"):
        a1 = in1.ap if in1_ap is None else in1_ap
        return pg.add(pl(eng), lambda v: v.tensor_tensor(out=out.ap, in0=in0.ap, in1=a1, op=op),
                      reads=[in0, in1], writes=[out])

    def stt(out, in0, scalar, in1, op0, op1, eng="dve"):
        if isinstance(scalar, View):
            return pg.add(pl(eng), lambda v: v.scalar_tensor_tensor(out=out.ap, in0=in0.ap, scalar=scalar.ap, in1=in1.ap,
                                                                    op0=op0, op1=op1),
                          reads=[in0, scalar, in1], writes=[out])
        return pg.add(pl(eng), lambda v: v.scalar_tensor_tensor(out=out.ap, in0=in0.ap, scalar=scalar, in1=in1.ap,
                                                                op0=op0, op1=op1),
                      reads=[in0, in1], writes=[out])

    def ts(out, in0, scalar, op, eng="dve"):
        if isinstance(scalar, View):
            return pg.add(pl(eng), lambda v: v.tensor_scalar(out=out.ap, in0=in0.ap, scalar1=scalar.ap, scalar2=None, op0=op),
                          reads=[in0, scalar], writes=[out])
        return pg.add(pl(eng), lambda v: v.tensor_scalar(out=out.ap, in0=in0.ap, scalar1=scalar, scalar2=None, op0=op),
                      reads=[in0], writes=[out])

    def vcopy(out, in_):
        return pg.add("dve", lambda v: v.tensor_copy(out=out.ap, in_=in_.ap), reads=[in_], writes=[out])

    def vmemset(out, val):
        return pg.add("dve", lambda v: v.memset(out.ap, val), writes=[out])

    def vrecip(out, in_):
        return pg.add("dve", lambda v: v.reciprocal(out=out.ap, in_=in_.ap), reads=[in_], writes=[out])

    def dma_in(eng, out, src_ap):
        if eng == "pool":
            return pg.add("pool", lambda g: g.dma_start(out=out.ap, in_=src_ap), writes=[out], dma=True)
        return pg.add("sp", lambda s: s.dma_start(out=out.ap, in_=src_ap), writes=[out], dma=True)

    def dma_out(dst_ap, src):
        return pg.add("act", lambda s: s.dma_start(out=dst_ap, in_=src.ap), reads=[src], dma=True)

    def wload(src_ap, shape, key):
        n = P * int(np.prod(shape))
        if key not in wreg:
            off = wscr_cur[0]
            wscr_cur[0] += n
            assert wscr_cur[0] <= WSCR_ELEMS
            if len(shape) == 2:
                dst = wscr[off:off + n].rearrange("(p k c) -> p k c", p=P, k=shape[0])
            else:
                dst = wscr[off:off + n].rearrange("(p c) -> p c", p=P)
            cops = []
            nk = shape[0]
            step = 8 if nk > 8 else nk
            for k0 in range(0, nk, step):
                k1 = min(nk, k0 + step)
                d_ = dst[:, k0:k1]
                s_ = src_ap[:, k0:k1]
                cops.append(pg.add("pool", lambda g, d_=d_, s_=s_: g.dma_start(out=d_, in_=s_), dma=True))
            wreg[key] = (off, cops, dst)
        off, cops, dst = wreg[key]
        wb = walloc(shape)
        op = dma_in("sp", wb.all(), dst)
        op.deps.update(cops)
        return wb

    def wview(w, l, c0, c1):
        return w[l].rearrange("(k p) n -> p k n", p=P)[:, :, c0:c1]

    vmemset(S.all(), 0.0)
    vmemset(Sb.all(), 0.0)
    vmemset(HALO.all(), 0.0)
    vmemset(EPSC.all(), EPS)
    vmemset(LOADT.all(), 0.0)
    dma_in("sp", IDF.all(), c_ident)
    dma_in("pool", IDB.all(), c_ident)
    dma_in("sp", MASK.all(), c_mask)
    dma_in("sp", DQ.all(), c_dq)
    dma_in("sp", DKH.all(), c_dk)
    t_ones = TMPA.next()
    vmemset(t_ones[0:P], 1.0 / D)
    vcopy(ONES_D.all(), t_ones[0:P])
    t_ones = TMPA.next()
    vmemset(t_ones[0:P], 1.0 / DV)
    vcopy(ONES_V.all(), t_ones[0:P])
    xs0 = XS.next()
    dma_in("sp", xs0[0:P], ng_d.rearrange("l n (c p) -> (l n c) p", p=P))
    b = ps_next()
    tr(PSB[b, 0:P], xs0[0:P], IDF.all())
    vcopy(G.all(), PSB[b, 0:P])
    ts(GH.all(), G.all(), 0.5, ALU.mult)
    dma_in("sp", Buf(LOADT.ap[0:24], "sb", LOADT.off, [P], 4).all(), w_cw[0].rearrange("k (c p) -> (k c) p", p=P))
    b = ps_next()
    tr(PSB[b, 0:P], LOADT.all(), IDF.all())
    vcopy(CW.all(), PSB[b, 0:P])

    def gcol(Gb, l, n, c):
        j = (l * 8 + n) * 8 + c
        return Gb[j:j + 1]

    def rstd_from(ssb):
        r = RSTD.next()
        act(r.all(), PSB[ssb], AF.Sqrt, bias=EPSC.all())
        vrecip(r.all(), r.all())
        return r

    def prenorm(l, n):
        pg.tag = pg.tag.split('.')[0] + '.pre'
        ssb = ss_next()
        for c in range(KC):
            sq = SQ.next()
            act(sq.all(), X[c], AF.Square)
            mm(PSB[ssb], ONES_D.all(), sq.all(), c == 0, c == KC - 1)
        r = rstd_from(ssb)
        for c in range(KC):
            if c >= 5 and cur_tile[0] > 0:
                tmp = TMPB.next()
                tt(tmp.all(), X[c], r.all(), ALU.mult, eng="pool")
                act(XN[c], tmp.all(), AF.Copy, scale=gcol(G, l, n, c))
            else:
                stt(XN[c], X[c], gcol(G, l, n, c), r.all(), ALU.mult, ALU.mult)

    class PostNorm:
        def __init__(self, l, n, half):
            self.ssb = ss_next()
            self.n = 0
            self.pending = None
            self.l, self.nn = l, n
            self.Gb = GH if half else G
            self.scaled = True

        def _flush(self):
            if self.pending is not None:
                sq = self.pending
                self.pending = None
                mm(PSB[self.ssb], ONES_D.all(), sq.all(), self.n == 0, self.n == KC - 1)
                self.n += 1

        def add_from_psum(self, j, psv):
            self._flush()
            act(Y[j], psv, AF.Copy, scale=gcol(self.Gb, self.l, self.nn, j))
            sq = SQ.next()
            act(sq.all(), psv, AF.Square)
            self.pending = sq

        def add_from_y(self, j):
            self.scaled = False
            self._flush()
            sq = SQ.next()
            act(sq.all(), Y[j], AF.Square)
            self.pending = sq

        def finish(self):
            self._flush()
            pg.tag = pg.tag.split('.')[0] + '.post'
            assert self.n == KC
            r = rstd_from(self.ssb)
            for j in range(KC):
                t = TMPA.next()
                if self.scaled:
                    e = "pool" if j in (1, 4, 7) else "dve"
                    tt(t.all(), Y[j], r.all(), ALU.mult, eng=e)
                    tt(X[j], t.all(), X[j], ALU.add, eng=e)
                else:
                    tt(t.all(), Y[j], r.all(), ALU.mult, eng="pool")
                    stt(X[j], t.all(), gcol(self.Gb, self.l, self.nn, j), X[j], ALU.mult, ALU.add)

    def ffn(l, wg, wu, wd, n_pre, n_post, tag):
        pg.tag = tag
        prenorm(l, n_pre)
        pg.tag = tag + '.gu'
        tiles_ = {}

        def gu_tile(f2):
            if f2 not in tiles_:
                tiles_[f2] = (wload(wview(wg, l, f2 * 256, (f2 + 1) * 256), [KC, 256], (tag, 'g', l, f2)),
                              wload(wview(wu, l, f2 * 256, (f2 + 1) * 256), [KC, 256], (tag, 'u', l, f2)))
            return tiles_[f2]

        def gu_evac(f, bg, bu):
            sg = TMPB.next()
            act(sg.all(), PSB[bg], AF.Silu)
            tt(Hh[f], sg.all(), PSB[bu], ALU.mult)

        head = [(0, 0), (0, 1), (1, 0)]
        hb_ = []
        for (f2, hf) in head:
            gu_tile(f2)
            hb_.append((ps_next(), ps_next()))
        for k in range(KC):
            for (f2, hf), (bg, bu) in zip(head, hb_):
                g_t, u_t = tiles_[f2]
                mm(PSB[bg], g_t[k, hf * P:(hf + 1) * P], XN[k], k == 0, k == KC - 1)
                mm(PSB[bu], u_t[k, hf * P:(hf + 1) * P], XN[k], k == 0, k == KC - 1)
        for (f2, hf), (bg, bu) in zip(head, hb_):
            gu_evac(2 * f2 + hf, bg, bu)
        for f2 in range(FC // 2):
            for hf in range(2):
                if (f2, hf) in head:
                    continue
                g_t, u_t = gu_tile(f2)
                bg, bu = ps_next(), ps_next()
                for k in range(KC):
                    mm(PSB[bg], g_t[k, hf * P:(hf + 1) * P], XN[k], k == 0, k == KC - 1)
                for k in range(KC):
                    mm(PSB[bu], u_t[k, hf * P:(hf + 1) * P], XN[k], k == 0, k == KC - 1)
                gu_evac(2 * f2 + hf, bg, bu)
        pg.tag = tag + '.dn'
        pn = PostNorm(l, n_post, True)
        for j2 in range(KC // 2):
            d_t = wload(wd[l].rearrange("(k p) n -> p k n", p=P)[:, :, j2 * 256:(j2 + 1) * 256], [FC, 256], (tag, 'd', l, j2))
            for hf in range(2):
                j = 2 * j2 + hf
                by = ps_next()
                for f in range(FC):
                    mm(PSB[by], d_t[f, hf * P:(hf + 1) * P], Hh[f], f == 0, f == FC - 1)
                pn.add_from_psum(j, PSB[by])
        pn.finish()

    def conv_mixer(l):
        pg.tag = 'conv'
        prenorm(l, MIX_PRE)
        pg.tag = 'conv.in'
        for m2 in range(KC // 2):
            wb_ = wload(wview(w_cin, 0, m2 * 256, (m2 + 1) * 256), [KC, 256], ('cb', m2))
            wc_ = wload(wview(w_cin, 0, D + m2 * 256, D + (m2 + 1) * 256), [KC, 256], ('cc', m2))
            wh_ = wload(wview(w_cin, 0, 2 * D + m2 * 256, 2 * D + (m2 + 1) * 256), [KC, 256], ('ch', m2))
            for hf in range(2):
                m = 2 * m2 + hf
                bb, bc, bh = ps_next(), ps_next(), ps_next()
                for k in range(KC):
                    mm(PSB[bc], wc_[k, hf * P:(hf + 1) * P], XN[k], k == 0, k == KC - 1)
                for k in range(KC):
                    mm(PSB[bh], wh_[k, hf * P:(hf + 1) * P], XN[k], k == 0, k == KC - 1)
                for k in range(KC):
                    mm(PSB[bb], wb_[k, hf * P:(hf + 1) * P], XN[k], k == 0, k == KC - 1)
                cs = TMPB.next()
                act(cs.all(), PSB[bc], AF.Copy)
                bsb = TMPB.next()
                act(bsb.all(), PSB[bb], AF.Copy)
                vcopy(U[m, 0:2], HALO[m])
                tt(U[m, 2:TT + 2], cs.all(), PSB[bh], ALU.mult)
                v = TMPA.next()
                act(v.all(), U[m, 2:TT + 2], AF.Copy, scale=CW[16 + m:17 + m])
                stt(v.all(), U[m, 1:TT + 1], CW[8 + m:9 + m], v.all(), ALU.mult, ALU.add)
                stt(v.all(), U[m, 0:TT], CW[m:m + 1], v.all(), ALU.mult, ALU.add)
                tt(BV[m], v.all(), bsb.all(), ALU.mult, eng="pool")
                vcopy(HALO[m], U[m, TT:TT + 2])
        pg.tag = 'conv.out'
        pn = PostNorm(l, MIX_POST, False)
        for j2 in range(KC // 2):
            o_t = wload(wview(w_cout, 0, j2 * 256, (j2 + 1) * 256), [KC, 256], ('co', j2))
            for hf in range(2):
                j = 2 * j2 + hf
                by = ps_next()
                for m in range(KC):
                    mm(PSB[by], o_t[m, hf * P:(hf + 1) * P], BV[m], m == 0, m == KC - 1)
                pn.add_from_psum(j, PSB[by])
        pn.finish()

    GAMMA = [1.0 - 2.0 ** (-5 - h) for h in range(H)]
    GAMMA_C = [float(np.float32(g) ** np.float32(CH)) for g in GAMMA]

    def retention(l):
        pg.tag = 'ret'
        prenorm(l, MIX_PRE)
        pg.tag = 'ret.qk'
        qoff, koff, voff, goff = 0, H * DK, 2 * H * DK, 2 * H * DK + H * DV
        for h in range(H):
            for which in range(2):
                base = (qoff if which == 0 else koff) + h * DK
                w_t = wload(wview(w_rin, 0, base, base + DK), [KC, DK], ('rqk', h, which))
                b1, b2 = ps_next(), ps_next()
                for k in range(KC):
                    mm(PSB[b1], w_t[k, 0:P], XN[k], k == 0, k == KC - 1)
                for k in range(KC):
                    mm(PSB[b2], w_t[k, P:2 * P], XN[k], k == 0, k == KC - 1)
                dst = Q if which == 0 else Kb
                a = TMPA.next()
                bt = TMPB.next()
                tt(a.all(), PSB[b1], CS[0], ALU.mult)
                tt(bt.all(), PSB[b2], CS[1], ALU.mult)
                tt(dst[2 * h], a.all(), bt.all(), ALU.subtract, eng="pool")
                a = TMPA.next()
                bt = TMPB.next()
                tt(a.all(), PSB[b2], CS[0], ALU.mult)
                tt(bt.all(), PSB[b1], CS[1], ALU.mult)
                tt(dst[2 * h + 1], a.all(), bt.all(), ALU.add, eng="pool")
                if which == 0:
                    dq_b = DQ[h].ap.unsqueeze(1).to_broadcast([P, NCH, P])
                    for hh in range(2):
                        qd3 = QD[2 * h + hh].ap.rearrange("p (a b) -> p a b", a=NCH)
                        q3 = Q[2 * h + hh].ap.rearrange("p (a b) -> p a b", a=NCH)
                        pg.add(pl("pool"), lambda v, qd3=qd3, q3=q3, dq_b=dq_b: v.tensor_tensor(out=qd3, in0=q3, in1=dq_b, op=ALU.mult),
                               reads=[Q[2 * h + hh], DQ[h]], writes=[QD[2 * h + hh]])
        pg.tag = 'ret.v'
        for vb in range(H):
            w_t = wload(wview(w_rin, 0, voff + vb * DV, voff + (vb + 1) * DV), [KC, DV], ('rv', vb))
            for n in range(NCH):
                bv_ = ps_next()
                for k in range(KC):
                    mm(PSB[bv_], XN[k, n * P:(n + 1) * P], w_t[k], k == 0, k == KC - 1)
                act(V[n, vb * DV:(vb + 1) * DV], PSB[bv_], AF.Copy)
        pg.tag = 'ret.core'
        for n in range(NCH):
            tsl = slice(n * P, (n + 1) * P)
            sts, kts, bos, sqs = [], [], [], []
            for h in range(H):
                bs = ps_next()
                for dc in range(2):
                    mm(PSB[bs, 0:P], Kb[2 * h + dc, tsl], Q[2 * h + dc, tsl], dc == 0, dc == 1)
                for dc in range(2):
                    tr(PSB16[bs, 512 + dc * P:512 + (dc + 1) * P], Kb[2 * h + dc, tsl], IDB.all())
                st_ = ST_R.next()
                s_ap = PSB[bs, 0:P].ap
                pg.add("dve", lambda v, o=st_.all().ap, i0=s_ap, i1=MASK[h].ap: v.tensor_tensor(out=o, in0=i0, in1=i1, op=ALU.mult),
                       reads=[PSB[bs], MASK[h]], writes=[st_.all()])
                kt = KT_R.next()
                k_ap = PSB16[bs, 512:512 + 2 * P].ap
                pg.add("dve", lambda v, o=kt.all().ap, i0=k_ap, sc=DKH[h:h + 1].ap: v.tensor_scalar(out=o, in0=i0, scalar1=sc, scalar2=None, op0=ALU.mult),
                       reads=[PSB[bs], DKH[h:h + 1]], writes=[kt.all()])
                sts.append(st_)
                kts.append(kt)
            for h in range(H):
                bo = ps_next()
                for ec in range(4):
                    osl = slice(ec * P, (ec + 1) * P)
                    mm(PSB[bo, osl], V[n, h * DV + ec * P:h * DV + (ec + 1) * P], sts[h].all(), True, False)
                    for dc in range(2):
                        mm(PSB[bo, osl], Sb[2 * h + dc, ec * P:(ec + 1) * P], QD[2 * h + dc, tsl], False, dc == 1)
                sq = SQ.next()
                act(sq.all(), PSB[bo], AF.Square)
                bos.append(bo)
                sqs.append(sq)
            for h in range(H):
                bo, sq = bos[h], sqs[h]
                bgn = ss_next()
                for ec in range(4):
                    mm(PSB[bgn, 0:P], ONES_V.all(), sq[ec * P:(ec + 1) * P], ec == 0, ec == 3)
                rs = RSG_R.next()
                act(rs.all(), PSB[bgn, 0:P], AF.Sqrt, bias=EPSC.all())
                vrecip(rs.all(), rs.all())
                rs_b = rs.all().ap.unsqueeze(1).to_broadcast([P, 4, P])
                o3 = PSB[bo].ap.rearrange("p (a b) -> p a b", a=4)
                yr3 = YR[4 * h:4 * h + 4, tsl]
                pg.add("dve", lambda v, yr3=yr3, o3=o3, rs_b=rs_b: v.tensor_tensor(out=yr3.ap, in0=o3, in1=rs_b, op=ALU.mult),
                       reads=[PSB[bo], rs.all()], writes=[yr3])
            for h in range(H):
                for dc in range(2):
                    bd = ps_next()
                    mm(PSB[bd], kts[h][dc * P:(dc + 1) * P], V[n, h * DV:(h + 1) * DV], True, True)
                    stt(S[2 * h + dc], S[2 * h + dc], GAMMA_C[h], PSB[bd], ALU.mult, ALU.add)
                    act(Sb[2 * h + dc], S[2 * h + dc], AF.Copy)
        pg.tag = 'ret.g'
        for gb in range(H * DV // 256):
            w_t = wload(wview(w_rin, 0, goff + gb * 256, goff + (gb + 1) * 256), [KC, 256], ('rg', gb))
            for hf in range(2):
                c = 2 * gb + hf
                bg = ps_next()
                for k in range(KC):
                    mm(PSB[bg], w_t[k, hf * P:(hf + 1) * P], XN[k], k == 0, k == KC - 1)
                sgt = TMPB.next()
                act(sgt.all(), PSB[bg], AF.Silu)
                tt(YR[c], YR[c], sgt.all(), ALU.mult)
        pg.tag = 'ret.out'
        pn = PostNorm(l, MIX_POST, False)
        for j2 in range(KC // 2):
            o_t = wload(wview(w_rout, 0, j2 * 256, (j2 + 1) * 256), [H * 4, 256], ('ro', j2))
            for hf in range(2):
                j = 2 * j2 + hf
                by = ps_next()
                for c in range(H * 4):
                    mm(PSB[by], o_t[c, hf * P:(hf + 1) * P], YR[c], c == 0, c == H * 4 - 1)
                pn.add_from_psum(j, PSB[by])
        pn.finish()

    def ple(l):
        pg.tag = 'ple'
        prenorm(l, PLE_PRE)
        pg.tag = 'ple.main'
        pn = PostNorm(l, PLE_POST, False)
        ptiles = {}

        def p_tile(j2):
            if j2 not in ptiles:
                ptiles[j2] = (wload(wview(w_pg, l, j2 * 256, (j2 + 1) * 256), [KC, 256], ('pg', l, j2)),
                              wload(wview(w_pp, l, j2 * 256, (j2 + 1) * 256), [2, 256], ('pp', l, j2)))
            return ptiles[j2]

        def p_evac(j, bg, be):
            sg = TMPB.next()
            act(sg.all(), PSB[bg], AF.Sigmoid)
            tt(Y[j], sg.all(), PSB[be], ALU.mult)
            pn.add_from_y(j)

        head = [(0, 0), (0, 1), (1, 0)]
        hb_ = []
        for (j2, hf) in head:
            p_tile(j2)
            hb_.append((ps_next(), ps_next()))
        for (j2, hf), (bg, be) in zip(head, hb_):
            p_t = ptiles[j2][1]
            for k in range(2):
                mm(PSB[be], p_t[k, hf * P:(hf + 1) * P], PT[l, k], k == 0, k == 1)
        for k in range(KC):
            for (j2, hf), (bg, be) in zip(head, hb_):
                g_t = ptiles[j2][0]
                mm(PSB[bg], g_t[k, hf * P:(hf + 1) * P], XN[k], k == 0, k == KC - 1)
        for (j2, hf), (bg, be) in zip(head, hb_):
            p_evac(2 * j2 + hf, bg, be)
        for j2 in range(KC // 2):
            for hf in range(2):
                if (j2, hf) in head:
                    continue
                g_t, p_t = p_tile(j2)
                j = 2 * j2 + hf
                bg, be = ps_next(), ps_next()
                for k in range(KC):
                    mm(PSB[bg], g_t[k, hf * P:(hf + 1) * P], XN[k], k == 0, k == KC - 1)
                for k in range(2):
                    mm(PSB[be], p_t[k, hf * P:(hf + 1) * P], PT[l, k], k == 0, k == 1)
                p_evac(j, bg, be)
        pn.finish()

    out_dmas = []

    def load_tile(ti):
        pg.tag = 'load'
        t0 = ti * TT
        for n in range(NCH):
            dma_in("sp", ISLOT[n].all(), x_d[t0 + n * P:t0 + (n + 1) * P, :])
        for n in range(NCH):
            xs = ISLOT[n]
            for hb in range(2):
                b = ps_next()
                for q in range(4):
                    c = hb * 4 + q
                    tr(PSB[b, q * P:(q + 1) * P], xs[c * P:(c + 1) * P], IDF.all())
                dst = X[hb * 4:hb * 4 + 4, n * P:(n + 1) * P]
                src = PSB[b].ap.rearrange("p (a b) -> p a b", a=4)
                pg.add("act", lambda a, dst=dst, src=src: a.activation(dst.ap, src, AF.Copy),
                       reads=[PSB[b]], writes=[dst])
            for l in layers:
                pst = PS_ST.next()
                dma_in("sp", pst.all(), p_d[l, t0 + n * P:t0 + (n + 1) * P, :])
                b = ps_next()
                for k in range(2):
                    tr(PSB[b, k * P:(k + 1) * P], pst[k * P:(k + 1) * P], IDF.all())
                dst = PT[l, 0:2, n * P:(n + 1) * P]
                src = PSB[b, 0:2 * P].ap.rearrange("p (a b) -> p a b", a=2)
                pg.add("dve", lambda v, dst=dst, src=src: v.tensor_copy(out=dst.ap, in_=src),
                       reads=[PSB[b, 0:2 * P]], writes=[dst])
        if 1 in layers:
            dma_in("sp", CS[0], c_cos[:, t0:t0 + TT])
            dma_in("sp", CS[1], c_sin[:, t0:t0 + TT])

    def store_tile(ti):
        pg.tag = 'store'
        t0 = ti * TT
        for n in range(NCH):
            xs = OSLOT[n]
            for hb in range(2):
                b = ps_next()
                for q in range(4):
                    c = hb * 4 + q
                    tr(PSB[b, q * P:(q + 1) * P], X[c, n * P:(n + 1) * P], IDF.all())
                act(xs[hb * 4 * P:(hb * 4 + 4) * P], PSB[b], AF.Copy)
            out_dmas.append(dma_out(y_d[t0 + n * P:t0 + (n + 1) * P, :], xs.all()))

    for ti in range(ntile):
        cur_tile[0] = ti
        load_tile(ti)
        for l in layers:
            ffn(l, w_f1g, w_f1u, w_f1d, FFN1_PRE, FFN1_POST, 'f1')
            if l % 2 == 0:
                conv_mixer(l)
            else:
                retention(l)
            ffn(l, w_f2g, w_f2u, w_f2d, FFN2_PRE, FFN2_POST, 'f2')
            ple(l)
        store_tile(ti)
    fin = pg.add("sp", None)
    fin.deps.update(out_dmas)

    pg.finalize()

    sem_cms = []
    sems = {}

    def mksem(key, name):
        cm = nc.semaphore(name)
        sem_cms.append(cm)
        sems[key] = cm.__enter__()

    for e in ("pe", "act", "dve", "pool", "sp"):
        mksem((e, "c"), "c_" + e)
    for e in ("pool", "sp", "act"):
        for s_ in range(Prog.NDMA_SEM):
            mksem((e, s_), "d_%s_%d" % (e, s_))

    with nc.Block() as block:
        @block.tensor
        def _(t):
            pg.emit_engine("pe", t, sems)

        @block.scalar
        def _(a):
            pg.emit_engine("act", a, sems)

        @block.vector
        def _(v):
            pg.emit_engine("dve", v, sems)

        @block.gpsimd
        def _(g):
            pg.emit_engine("pool", g, sems)

        @block.sync
        def _(s):
            pg.emit_engine("sp", s, sems)

    for cm in reversed(sem_cms):
        cm.__exit__(None, None, None)
    psum_cm.__exit__(None, None, None)
    arena_cm.__exit__(None, None, None)
    nstat = {e: len(pg.ops[e]) for e in Prog.ENGS}
    nstat["_pe_tags"] = [op.tag for op in pg.ops["pe"]]
    return nc, nstat


def make_consts(T):
    f32 = np.float32
    ident = np.eye(P, dtype=f32)
    gam = np.array([1.0 - 2.0 ** (-5 - h) for h in range(H)], dtype=np.float64)
    idx = np.arange(CH, dtype=np.float64)
    diff = idx[None, :] - idx[:, None]
    mask = np.zeros((P, H, P), dtype=f32)
    for h in range(H):
        m = np.where(diff >= 0, gam[h] ** np.maximum(diff, 0.0), 0.0) * (DK ** -0.5)
        mask[:, h, :] = m.astype(f32)
    dq = np.zeros((P, H, P), dtype=f32)
    for h in range(H):
        row = gam[h] ** (idx + 1.0)
        dq[:, h, :] = row.astype(f32)[None, :]
    dk = np.zeros((P, H), dtype=f32)
    for h in range(H):
        dk[:, h] = (gam[h] ** (CH - 1.0 - idx) * (DK ** -0.5)).astype(f32)
    half = DK // 2
    inv_freq = (1.0 / (np.float32(10000.0) ** np.linspace(0.0, 1.0, half, dtype=f32))).astype(f32)
    pos = np.arange(T, dtype=f32)
    ang = (inv_freq[:, None] * pos[None, :]).astype(f32)
    return {
        "c_ident": ident, "c_mask": mask, "c_dq": dq, "c_dk": dk,
        "c_cos": np.cos(ang).astype(f32), "c_sin": np.sin(ang).astype(f32),
    }


_WNAMES = ["norm_g", "ffn1_w_gate", "ffn1_w_up", "ffn1_w_down", "ffn2_w_gate", "ffn2_w_up", "ffn2_w_down",
           "conv_w_in", "conv_w", "conv_w_out", "ret_w_in", "ret_w_out", "ple_w_proj", "ple_w_gate"]


def run(inputs, T, n_cores, layers=(0, 1), trace=False):
    nc, _ = build(T, layers)
    consts = make_consts(T)
    shared = {k: np.ascontiguousarray(np.asarray(inputs[k], dtype=np.float32)) for k in _WNAMES}
    shared.update(consts)
    x = np.asarray(inputs["x"], dtype=np.float32)
    p = np.asarray(inputs["p"], dtype=np.float32)
    in_maps = []
    for c in range(n_cores):
        m = dict(shared)
        m["x"] = np.ascontiguousarray(x[c])
        m["p"] = np.ascontiguousarray(p[:, c])
        in_maps.append(m)
    res = run_bass_kernel_spmd(nc, in_maps, core_ids=list(range(n_cores)), trace=trace)
    out = np.stack([np.asarray(r["y"]) for r in res.results], axis=0)
    return out, res


def kernel(**inputs):
    out, _ = run(inputs, SEQ, N_CORES)
    return out.astype(np.float32)
```

```python
import numpy as np
import concourse.bass as bass
import concourse.mybir as mybir
from concourse.bass_utils import run_bass_kernel_spmd

F32 = mybir.dt.float32
BF16 = mybir.dt.bfloat16
AF = mybir.ActivationFunctionType
ALU = mybir.AluOpType

P = 128
D = 1024
KC = D // P
DFF = 2816
FC = DFF // P
DPLE = 256
TT = 512
NCH = TT // P
EPS = 1e-6
H = 4
DK = 256
DV = 512
CH = 128
N_CORES = 8
SEQ = 4096
BLK = 256

FFN1_PRE, FFN1_POST, MIX_PRE, MIX_POST, FFN2_PRE, FFN2_POST, PLE_PRE, PLE_POST = range(8)


class View:
    __slots__ = ("ap", "blocks")

    def __init__(self, ap, blocks):
        self.ap = ap
        self.blocks = blocks


class Buf:
    def __init__(self, ap, space, off, shape, esz):
        self.ap, self.space, self.off, self.shape, self.esz = ap, space, off, tuple(shape), esz
        st, acc = [], 1
        for n in reversed(self.shape):
            st.append(acc)
            acc *= n
        self.strides = tuple(reversed(st))
        self.nbytes = acc * esz

    def __getitem__(self, idx):
        if not isinstance(idx, tuple):
            idx = (idx,)
        idx = idx + (slice(None),) * (len(self.shape) - len(idx))
        rng = []
        for i, n in zip(idx, self.shape):
            if isinstance(i, int):
                rng.append((i, i + 1))
            else:
                a, b, s = i.indices(n)
                assert s == 1
                rng.append((a, b))
        L = 1
        d = len(rng) - 1
        while d >= 0:
            a, b = rng[d]
            if a == 0 and b == self.shape[d]:
                L *= self.shape[d]
                d -= 1
                continue
            break
        starts = [0]
        if d >= 0:
            a, b = rng[d]
            L *= (b - a)
            starts = [a * self.strides[d]]
            for dd in range(d - 1, -1, -1):
                a, b = rng[dd]
                starts = [s0 + i * self.strides[dd] for i in range(a, b) for s0 in starts]
        blocks = set()
        for s0 in starts:
            lo = (self.off + s0 * self.esz) // BLK
            hi = (self.off + (s0 + L) * self.esz - 1) // BLK
            for b in range(lo, hi + 1):
                blocks.add((self.space, b))
        return View(self.ap[(slice(None),) + idx], blocks)

    def all(self):
        return self[tuple(slice(None) for _ in self.shape)]


class Op:
    __slots__ = ("eng", "fn", "deps", "sig", "sem", "val", "inc", "is_dma", "tag")

    def __init__(self, eng, fn, is_dma):
        self.eng, self.fn, self.is_dma = eng, fn, is_dma
        self.deps = set()
        self.sig = is_dma
        self.sem = None
        self.val = 0
        self.inc = 16 if is_dma else 1


class Prog:
    ENGS = ("pe", "act", "dve", "pool", "sp")
    NDMA_SEM = 12
    NSEM = {"pool": 3}

    def __init__(self):
        self.ops = {e: [] for e in self.ENGS}
        self.state = {}
        self.dma_count = {e: 0 for e in self.ENGS}
        self.dma_last = {}
        self.tag = ""

    def add(self, eng, fn, reads=(), writes=(), dma=False):
        op = Op(eng, fn, dma)
        op.tag = self.tag
        deps = op.deps
        st = self.state
        for v in reads:
            for b in v.blocks:
                e = st.get(b)
                if e is not None and e[0] is not None:
                    deps.add(e[0])
        for v in writes:
            for b in v.blocks:
                e = st.get(b)
                if e is not None:
                    if e[0] is not None:
                        deps.add(e[0])
                    deps.update(e[1])
        for v in reads:
            for b in v.blocks:
                e = st.get(b)
                if e is None:
                    st[b] = [None, [op]]
                else:
                    e[1].append(op)
        for v in writes:
            for b in v.blocks:
                st[b] = [op, []]
        deps.discard(op)
        if dma:
            n = self.dma_count[eng]
            self.dma_count[eng] = n + 1
            nsem = self.NSEM.get(eng, self.NDMA_SEM)
            slot = n % nsem
            prev = self.dma_last.get((eng, slot))
            if prev is not None:
                deps.add(prev)
            self.dma_last[(eng, slot)] = op
            op.sem = (eng, slot)
            op.val = 16 * (n // nsem + 1)
        self.ops[eng].append(op)
        return op

    def finalize(self):
        for e in self.ENGS:
            for op in self.ops[e]:
                for d in op.deps:
                    if d.is_dma:
                        continue
                    if d.eng == "pe" and e == "pe":
                        continue
                    d.sig = True
        for e in self.ENGS:
            cnt = 0
            for op in self.ops[e]:
                if op.is_dma:
                    continue
                if op.sig:
                    cnt += 1
                    op.sem = (e, "c")
                    op.val = cnt

    def emit_engine(self, ename, eng, sems):
        seen = {}
        for op in self.ops[ename]:
            need = {}
            for d in op.deps:
                if (not d.is_dma) and d.eng == "pe" and ename == "pe":
                    continue
                if d.val > need.get(d.sem, 0):
                    need[d.sem] = d.val
            for k, v in need.items():
                if seen.get(k, 0) >= v:
                    continue
                eng.wait_ge(sems[k], v)
                seen[k] = v
            if op.fn is not None:
                ins = op.fn(eng)
                if op.sig:
                    ins.then_inc(sems[op.sem], op.inc)


class Ring:
    def __init__(self, bufs):
        self.bufs = bufs
        self.i = 0

    def next(self):
        b = self.bufs[self.i % len(self.bufs)]
        self.i += 1
        return b


def build(T=SEQ, layers=(0, 1)):
    nc = bass.Bass("TRN2", target_bir_lowering=False)
    ntile = T // TT
    pg = Prog()

    def din(name, shape):
        return nc.dram_tensor(name, list(shape), F32, kind="ExternalInput").ap()

    x_d = din("x", [T, D])
    p_d = din("p", [2, T, DPLE])
    ng_d = din("norm_g", [2, 8, D])
    w_f1g = din("ffn1_w_gate", [2, D, DFF])
    w_f1u = din("ffn1_w_up", [2, D, DFF])
    w_f1d = din("ffn1_w_down", [2, DFF, D])
    w_f2g = din("ffn2_w_gate", [2, D, DFF])
    w_f2u = din("ffn2_w_up", [2, D, DFF])
    w_f2d = din("ffn2_w_down", [2, DFF, D])
    w_cin = din("conv_w_in", [1, D, 3 * D])
    w_cw = din("conv_w", [1, 3, D])
    w_cout = din("conv_w_out", [1, D, D])
    w_rin = din("ret_w_in", [1, D, 2 * H * DK + 2 * H * DV])
    w_rout = din("ret_w_out", [1, H * DV, D])
    w_pp = din("ple_w_proj", [2, DPLE, D])
    w_pg = din("ple_w_gate", [2, D, D])
    c_ident = din("c_ident", [P, P])
    c_mask = din("c_mask", [P, H, P])
    c_dq = din("c_dq", [P, H, P])
    c_dk = din("c_dk", [P, H])
    c_cos = din("c_cos", [P, T])
    c_sin = din("c_sin", [P, T])
    y_d = nc.dram_tensor("y", [T, D], F32, kind="ExternalOutput").ap()
    WSCR_ELEMS = 2 * (2 * 3 * D * DFF) + D * 3 * D + D * D + D * (2 * H * DK + 2 * H * DV) + H * DV * D + 2 * (DPLE * D + D * D)
    wscr = nc.dram_tensor("wscr", [WSCR_ELEMS], BF16, kind="Internal").ap()
    wreg = {}
    wscr_cur = [0]
    cur_tile = [0]

    ARENA_BYTES = 206 * 1024
    arena_cm = nc.sbuf_tensor("arena", [P, ARENA_BYTES // 4], F32)
    psum_cm = nc.psum_tensor("ps", [P, 8, 512], F32)
    arena = arena_cm.__enter__()
    psum = psum_cm.__enter__()

    cursor = [0]

    def carve_at(off, shape, dt):
        esz = 4 if dt == F32 else 2
        n = int(np.prod(shape))
        nbytes = n * esz
        assert off % 4 == 0
        ap = arena[:, off // 4:(off + nbytes + 3) // 4]
        if dt == BF16:
            ap = ap.bitcast(BF16)
        if len(shape) == 2:
            ap = ap.rearrange("p (a b) -> p a b", a=shape[0])
        elif len(shape) == 3:
            ap = ap.rearrange("p (a b c) -> p a b c", a=shape[0], b=shape[1])
        return Buf(ap, "sb", off, shape, esz)

    def carve(shape, dt):
        esz = 4 if dt == F32 else 2
        nbytes = int(np.prod(shape)) * esz
        off = cursor[0]
        cursor[0] = (off + nbytes + BLK - 1) // BLK * BLK
        assert cursor[0] <= ARENA_BYTES, ("arena overflow", cursor[0])
        return carve_at(off, shape, dt)

    X = carve([KC, TT], F32)
    S = carve([H * 2, DV], F32)
    Sb = carve([H * 2, DV], BF16)
    HALO = carve([KC, 2], F32)
    IDF = carve([P], F32)
    IDB = carve([P], BF16)
    ONES_D = carve([P], BF16)
    ONES_V = carve([P], BF16)
    G = carve([P], F32)
    GH = carve([P], F32)
    CW = carve([P], F32)
    MASK = carve([H, P], F32)
    DQ = carve([H, P], F32)
    DKH = carve([H], F32)
    EPSC = carve([1], F32)
    PT = carve([2, 2, TT], BF16)
    CS = carve([2, TT], F32)
    XS = Ring([carve([D], F32) for _ in range(2)])
    PS_ST = Ring([carve([DPLE], F32) for _ in range(2)])
    LOADT = carve([P], F32)
    XN = carve([KC, TT], BF16)
    RSTD = Ring([carve([TT], F32) for _ in range(2)])
    SQ = Ring([carve([TT], BF16) for _ in range(6)])
    TMPA = Ring([carve([TT], F32) for _ in range(3)])
    TMPB = Ring([carve([TT], F32) for _ in range(3)])
    ST_R = Ring([carve([P], BF16) for _ in range(5)])
    KT_R = Ring([carve([2 * P], BF16) for _ in range(5)])
    RSG_R = Ring([carve([P], F32) for _ in range(3)])
    WBYTES = 40 * 1024
    w_base = cursor[0]
    cursor[0] += WBYTES
    scr0 = cursor[0]
    Hh = carve_at(scr0, [FC, TT], BF16)
    Y = carve_at(scr0 + 22 * 1024, [KC, TT], F32)
    U = carve_at(scr0, [KC, TT + 2], F32)
    BV = carve_at(scr0 + 38 * 1024, [KC, TT], BF16)
    YR = carve_at(scr0, [H * 4, TT], BF16)
    Q = carve_at(scr0 + 16 * 1024, [H * 2, TT], BF16)
    QD = carve_at(scr0 + 24 * 1024, [H * 2, TT], BF16)
    Kb = carve_at(scr0 + 32 * 1024, [H * 2, TT], BF16)
    V = carve_at(scr0 + 40 * 1024, [NCH, H * DV], BF16)
    assert scr0 + 56 * 1024 <= ARENA_BYTES, scr0
    OSLOT = [carve_at(scr0 + i * 4096, [D], F32) for i in range(NCH)]
    ISLOT = [carve_at(scr0 + 16 * 1024 + i * 4096, [D], F32) for i in range(NCH)]
    print("sbuf bytes used", scr0 + 56 * 1024)

    PSB = Buf(psum, "ps", 0, [8, 512], 4)
    PSB16 = Buf(psum.bitcast(BF16), "ps", 0, [8, 1024], 2)
    ps_i = [0]

    def ps_next():
        b = ps_i[0] % 6
        ps_i[0] += 1
        return b

    ss_i = [0]

    def ss_next():
        b = 6 + ss_i[0] % 2
        ss_i[0] += 1
        return b

    w_cur = [0]

    def walloc(shape):
        nbytes = int(np.prod(shape)) * 2
        nb = (nbytes + BLK - 1) // BLK * BLK
        assert nb <= WBYTES
        if w_cur[0] + nb > WBYTES:
            w_cur[0] = 0
        off = w_base + w_cur[0]
        w_cur[0] += nb
        return carve_at(off, shape, BF16)

    def mm(out, lhsT, rhs, start, stop):
        return pg.add("pe", lambda t: t.matmul(out.ap, lhsT.ap, rhs.ap, start=start, stop=stop),
                      reads=[lhsT, rhs], writes=[out])

    def warm(n):
        for _ in range(n):
            pg.add("pe", lambda t: t.matmul(PSB[5].ap, ONES_D.all().ap, XN[0].ap, start=True, stop=True),
                   reads=[ONES_D.all()], writes=[])

    def tr(out, in_, ident):
        return pg.add("pe", lambda t: t.transpose(out.ap, in_.ap, ident.ap), reads=[in_, ident], writes=[out])

    def act(out, in_, func, scale=1.0, bias=None, extra_reads=()):
        rd = [in_] + list(extra_reads)
        if bias is not None:
            rd.append(bias)
        sc = scale.ap if isinstance(scale, View) else scale
        if isinstance(scale, View):
            rd.append(scale)
        if bias is not None:
            f = lambda a: a.activation(out.ap, in_.ap, func, bias=bias.ap, scale=sc)
        else:
            f = lambda a: a.activation(out.ap, in_.ap, func, scale=sc)
        return pg.add("act", f, reads=rd, writes=[out])

    def pl(e):
        if e == "pool" and cur_tile[0] == 0:
            return "dve"
        return e

    def tt(out, in0, in1, op, in1_ap=None, eng="dve"):
        a1 = in1.ap if in1_ap is None else in1_ap
        return pg.add(pl(eng), lambda v: v.tensor_tensor(out=out.ap, in0=in0.ap, in1=a1, op=op),
                      reads=[in0, in1], writes=[out])

    def stt(out, in0, scalar, in1, op0, op1, eng="dve"):
        if isinstance(scalar, View):
            return pg.add(pl(eng), lambda v: v.scalar_tensor_tensor(out=out.ap, in0=in0.ap, scalar=scalar.ap, in1=in1.ap,
                                                                    op0=op0, op1=op1),
                          reads=[in0, scalar, in1], writes=[out])
        return pg.add(pl(eng), lambda v: v.scalar_tensor_tensor(out=out.ap, in0=in0.ap, scalar=scalar, in1=in1.ap,
                                                                op0=op0, op1=op1),
                      reads=[in0, in1], writes=[out])

    def ts(out, in0, scalar, op, eng="dve"):
        if isinstance(scalar, View):
            return pg.add(pl(eng), lambda v: v.tensor_scalar(out=out.ap, in0=in0.ap, scalar1=scalar.ap, scalar2=None, op0=op),
                          reads=[in0, scalar], writes=[out])
        return pg.add(pl(eng), lambda v: v.tensor_scalar(out=out.ap, in0=in0.ap, scalar1=scalar, scalar2=None, op0=op),
                      reads=[in0], writes=[out])

    def vcopy(out, in_):
        return pg.add("dve", lambda v: v.tensor_copy(out=out.ap, in_=in_.ap), reads=[in_], writes=[out])

    def vmemset(out, val):
        return pg.add("dve", lambda v: v.memset(out.ap, val), writes=[out])

    def vrecip(out, in_):
        return pg.add("dve", lambda v: v.reciprocal(out=out.ap, in_=in_.ap), reads=[in_], writes=[out])

    def dma_in(eng, out, src_ap):
        if eng == "pool":
            return pg.add("pool", lambda g: g.dma_start(out=out.ap, in_=src_ap), writes=[out], dma=True)
        return pg.add("sp", lambda s: s.dma_start(out=out.ap, in_=src_ap), writes=[out], dma=True)

    def dma_out(dst_ap, src):
        return pg.add("act", lambda s: s.dma_start(out=dst_ap, in_=src.ap), reads=[src], dma=True)

    def wload(src_ap, shape, key):
        n = P * int(np.prod(shape))
        if key not in wreg:
            off = wscr_cur[0]
            wscr_cur[0] += n
            assert wscr_cur[0] <= WSCR_ELEMS
            if len(shape) == 2:
                dst = wscr[off:off + n].rearrange("(p k c) -> p k c", p=P, k=shape[0])
            else:
                dst = wscr[off:off + n].rearrange("(p c) -> p c", p=P)
            cops = []
            nk = shape[0]
            step = 8 if nk > 8 else nk
            for k0 in range(0, nk, step):
                k1 = min(nk, k0 + step)
                d_ = dst[:, k0:k1]
                s_ = src_ap[:, k0:k1]
                cops.append(pg.add("pool", lambda g, d_=d_, s_=s_: g.dma_start(out=d_, in_=s_), dma=True))
            wreg[key] = (off, cops, dst)
        off, cops, dst = wreg[key]
        wb = walloc(shape)
        op = dma_in("sp", wb.all(), dst)
        op.deps.update(cops)
        return wb

    def wview(w, l, c0, c1):
        return w[l].rearrange("(k p) n -> p k n", p=P)[:, :, c0:c1]

    vmemset(S.all(), 0.0)
    vmemset(Sb.all(), 0.0)
    vmemset(HALO.all(), 0.0)
    vmemset(EPSC.all(), EPS)
    vmemset(LOADT.all(), 0.0)
    dma_in("sp", IDF.all(), c_ident)
    dma_in("pool", IDB.all(), c_ident)
    dma_in("sp", MASK.all(), c_mask)
    dma_in("sp", DQ.all(), c_dq)
    dma_in("sp", DKH.all(), c_dk)
    t_ones = TMPA.next()
    vmemset(t_ones[0:P], 1.0 / D)
    vcopy(ONES_D.all(), t_ones[0:P])
    t_ones = TMPA.next()
    vmemset(t_ones[0:P], 1.0 / DV)
    vcopy(ONES_V.all(), t_ones[0:P])
    xs0 = XS.next()
    dma_in("sp", xs0[0:P], ng_d.rearrange("l n (c p) -> (l n c) p", p=P))
    b = ps_next()
    tr(PSB[b, 0:P], xs0[0:P], IDF.all())
    vcopy(G.all(), PSB[b, 0:P])
    ts(GH.all(), G.all(), 0.5, ALU.mult)
    dma_in("sp", Buf(LOADT.ap[0:24], "sb", LOADT.off, [P], 4).all(), w_cw[0].rearrange("k (c p) -> (k c) p", p=P))
    b = ps_next()
    tr(PSB[b, 0:P], LOADT.all(), IDF.all())
    vcopy(CW.all(), PSB[b, 0:P])

    def gcol(Gb, l, n, c):
        j = (l * 8 + n) * 8 + c
        return Gb[j:j + 1]

    def rstd_from(ssb):
        r = RSTD.next()
        act(r.all(), PSB[ssb], AF.Sqrt, bias=EPSC.all())
        vrecip(r.all(), r.all())
        return r

    def prenorm(l, n):
        pg.tag = pg.tag.split('.')[0] + '.pre'
        ssb = ss_next()
        for c in range(KC):
            sq = SQ.next()
            act(sq.all(), X[c], AF.Square)
            mm(PSB[ssb], ONES_D.all(), sq.all(), c == 0, c == KC - 1)
        r = rstd_from(ssb)
        for c in range(KC):
            if c >= 5 and cur_tile[0] > 0:
                tmp = TMPB.next()
                tt(tmp.all(), X[c], r.all(), ALU.mult, eng="pool")
                act(XN[c], tmp.all(), AF.Copy, scale=gcol(G, l, n, c))
            else:
                stt(XN[c], X[c], gcol(G, l, n, c), r.all(), ALU.mult, ALU.mult)

    class PostNorm:
        def __init__(self, l, n, half):
            self.ssb = ss_next()
            self.n = 0
            self.pending = None
            self.l, self.nn = l, n
            self.Gb = GH if half else G
            self.scaled = True

        def _flush(self):
            if self.pending is not None:
                sq = self.pending
                self.pending = None
                mm(PSB[self.ssb], ONES_D.all(), sq.all(), self.n == 0, self.n == KC - 1)
                self.n += 1

        def add_from_psum(self, j, psv):
            self._flush()
            act(Y[j], psv, AF.Copy, scale=gcol(self.Gb, self.l, self.nn, j))
            sq = SQ.next()
            act(sq.all(), psv, AF.Square)
            self.pending = sq

        def add_from_y(self, j):
            self.scaled = False
            self._flush()
            sq = SQ.next()
            act(sq.all(), Y[j], AF.Square)
            self.pending = sq

        def finish(self):
            self._flush()
            pg.tag = pg.tag.split('.')[0] + '.post'
            assert self.n == KC
            r = rstd_from(self.ssb)
            for j in range(KC):
                t = TMPA.next()
                if self.scaled:
                    e = "pool" if j in (1, 4, 7) else "dve"
                    tt(t.all(), Y[j], r.all(), ALU.mult, eng=e)
                    tt(X[j], t.all(), X[j], ALU.add, eng=e)
                else:
                    tt(t.all(), Y[j], r.all(), ALU.mult, eng="pool")
                    stt(X[j], t.all(), gcol(self.Gb, self.l, self.nn, j), X[j], ALU.mult, ALU.add)

    def ffn(l, wg, wu, wd, n_pre, n_post, tag):
        pg.tag = tag
        prenorm(l, n_pre)
        pg.tag = tag + '.gu'
        tiles_ = {}

        def gu_tile(f2):
            if f2 not in tiles_:
                tiles_[f2] = (wload(wview(wg, l, f2 * 256, (f2 + 1) * 256), [KC, 256], (tag, 'g', l, f2)),
                              wload(wview(wu, l, f2 * 256, (f2 + 1) * 256), [KC, 256], (tag, 'u', l, f2)))
            return tiles_[f2]

        def gu_evac(f, bg, bu):
            sg = TMPB.next()
            act(sg.all(), PSB[bg], AF.Silu)
            tt(Hh[f], sg.all(), PSB[bu], ALU.mult)

        head = [(0, 0), (0, 1), (1, 0)]
        hb_ = []
        for (f2, hf) in head:
            gu_tile(f2)
            hb_.append((ps_next(), ps_next()))
        for k in range(KC):
            for (f2, hf), (bg, bu) in zip(head, hb_):
                g_t, u_t = tiles_[f2]
                mm(PSB[bg], g_t[k, hf * P:(hf + 1) * P], XN[k], k == 0, k == KC - 1)
                mm(PSB[bu], u_t[k, hf * P:(hf + 1) * P], XN[k], k == 0, k == KC - 1)
        for (f2, hf), (bg, bu) in zip(head, hb_):
            gu_evac(2 * f2 + hf, bg, bu)
        for f2 in range(FC // 2):
            for hf in range(2):
                if (f2, hf) in head:
                    continue
                g_t, u_t = gu_tile(f2)
                bg, bu = ps_next(), ps_next()
                for k in range(KC):
                    mm(PSB[bg], g_t[k, hf * P:(hf + 1) * P], XN[k], k == 0, k == KC - 1)
                for k in range(KC):
                    mm(PSB[bu], u_t[k, hf * P:(hf + 1) * P], XN[k], k == 0, k == KC - 1)
                gu_evac(2 * f2 + hf, bg, bu)
        pg.tag = tag + '.dn'
        pn = PostNorm(l, n_post, True)
        for j2 in range(KC // 2):
            d_t = wload(wd[l].rearrange("(k p) n -> p k n", p=P)[:, :, j2 * 256:(j2 + 1) * 256], [FC, 256], (tag, 'd', l, j2))
            for hf in range(2):
                j = 2 * j2 + hf
                by = ps_next()
                for f in range(FC):
                    mm(PSB[by], d_t[f, hf * P:(hf + 1) * P], Hh[f], f == 0, f == FC - 1)
                pn.add_from_psum(j, PSB[by])
        pn.finish()

    def conv_mixer(l):
        pg.tag = 'conv'
        prenorm(l, MIX_PRE)
        pg.tag = 'conv.in'
        for m2 in range(KC // 2):
            wb_ = wload(wview(w_cin, 0, m2 * 256, (m2 + 1) * 256), [KC, 256], ('cb', m2))
            wc_ = wload(wview(w_cin, 0, D + m2 * 256, D + (m2 + 1) * 256), [KC, 256], ('cc', m2))
            wh_ = wload(wview(w_cin, 0, 2 * D + m2 * 256, 2 * D + (m2 + 1) * 256), [KC, 256], ('ch', m2))
            for hf in range(2):
                m = 2 * m2 + hf
                bb, bc, bh = ps_next(), ps_next(), ps_next()
                for k in range(KC):
                    mm(PSB[bc], wc_[k, hf * P:(hf + 1) * P], XN[k], k == 0, k == KC - 1)
                for k in range(KC):
                    mm(PSB[bh], wh_[k, hf * P:(hf + 1) * P], XN[k], k == 0, k == KC - 1)
                for k in range(KC):
                    mm(PSB[bb], wb_[k, hf * P:(hf + 1) * P], XN[k], k == 0, k == KC - 1)
                cs = TMPB.next()
                act(cs.all(), PSB[bc], AF.Copy)
                bsb = TMPB.next()
                act(bsb.all(), PSB[bb], AF.Copy)
                vcopy(U[m, 0:2], HALO[m])
                tt(U[m, 2:TT + 2], cs.all(), PSB[bh], ALU.mult)
                v = TMPA.next()
                act(v.all(), U[m, 2:TT + 2], AF.Copy, scale=CW[16 + m:17 + m])
                stt(v.all(), U[m, 1:TT + 1], CW[8 + m:9 + m], v.all(), ALU.mult, ALU.add)
                stt(v.all(), U[m, 0:TT], CW[m:m + 1], v.all(), ALU.mult, ALU.add)
                tt(BV[m], v.all(), bsb.all(), ALU.mult, eng="pool")
                vcopy(HALO[m], U[m, TT:TT + 2])
        pg.tag = 'conv.out'
        pn = PostNorm(l, MIX_POST, False)
        for j2 in range(KC // 2):
            o_t = wload(wview(w_cout, 0, j2 * 256, (j2 + 1) * 256), [KC, 256], ('co', j2))
            for hf in range(2):
                j = 2 * j2 + hf
                by = ps_next()
                for m in range(KC):
                    mm(PSB[by], o_t[m, hf * P:(hf + 1) * P], BV[m], m == 0, m == KC - 1)
                pn.add_from_psum(j, PSB[by])
        pn.finish()

    GAMMA = [1.0 - 2.0 ** (-5 - h) for h in range(H)]
    GAMMA_C = [float(np.float32(g) ** np.float32(CH)) for g in GAMMA]

    def retention(l):
        pg.tag = 'ret'
        prenorm(l, MIX_PRE)
        pg.tag = 'ret.qk'
        qoff, koff, voff, goff = 0, H * DK, 2 * H * DK, 2 * H * DK + H * DV
        for h in range(H):
            for which in range(2):
                base = (qoff if which == 0 else koff) + h * DK
                w_t = wload(wview(w_rin, 0, base, base + DK), [KC, DK], ('rqk', h, which))
                b1, b2 = ps_next(), ps_next()
                for k in range(KC):
                    mm(PSB[b1], w_t[k, 0:P], XN[k], k == 0, k == KC - 1)
                for k in range(KC):
                    mm(PSB[b2], w_t[k, P:2 * P], XN[k], k == 0, k == KC - 1)
                dst = Q if which == 0 else Kb
                a = TMPA.next()
                bt = TMPB.next()
                tt(a.all(), PSB[b1], CS[0], ALU.mult)
                tt(bt.all(), PSB[b2], CS[1], ALU.mult)
                tt(dst[2 * h], a.all(), bt.all(), ALU.subtract, eng="pool")
                a = TMPA.next()
                bt = TMPB.next()
                tt(a.all(), PSB[b2], CS[0], ALU.mult)
                tt(bt.all(), PSB[b1], CS[1], ALU.mult)
                tt(dst[2 * h + 1], a.all(), bt.all(), ALU.add, eng="pool")
                if which == 0:
                    dq_b = DQ[h].ap.unsqueeze(1).to_broadcast([P, NCH, P])
                    for hh in range(2):
                        qd3 = QD[2 * h + hh].ap.rearrange("p (a b) -> p a b", a=NCH)
                        q3 = Q[2 * h + hh].ap.rearrange("p (a b) -> p a b", a=NCH)
                        pg.add(pl("pool"), lambda v, qd3=qd3, q3=q3, dq_b=dq_b: v.tensor_tensor(out=qd3, in0=q3, in1=dq_b, op=ALU.mult),
                               reads=[Q[2 * h + hh], DQ[h]], writes=[QD[2 * h + hh]])
        pg.tag = 'ret.v'
        for vb in range(H):
            w_t = wload(wview(w_rin, 0, voff + vb * DV, voff + (vb + 1) * DV), [KC, DV], ('rv', vb))
            for n in range(NCH):
                bv_ = ps_next()
                for k in range(KC):
                    mm(PSB[bv_], XN[k, n * P:(n + 1) * P], w_t[k], k == 0, k == KC - 1)
                act(V[n, vb * DV:(vb + 1) * DV], PSB[bv_], AF.Copy)
        pg.tag = 'ret.core'
        for n in range(NCH):
            tsl = slice(n * P, (n + 1) * P)
            sts, kts, bos, sqs = [], [], [], []
            for h in range(H):
                bs = ps_next()
                for dc in range(2):
                    mm(PSB[bs, 0:P], Kb[2 * h + dc, tsl], Q[2 * h + dc, tsl], dc == 0, dc == 1)
                for dc in range(2):
                    tr(PSB16[bs, 512 + dc * P:512 + (dc + 1) * P], Kb[2 * h + dc, tsl], IDB.all())
                st_ = ST_R.next()
                s_ap = PSB[bs, 0:P].ap
                pg.add("dve", lambda v, o=st_.all().ap, i0=s_ap, i1=MASK[h].ap: v.tensor_tensor(out=o, in0=i0, in1=i1, op=ALU.mult),
                       reads=[PSB[bs], MASK[h]], writes=[st_.all()])
                kt = KT_R.next()
                k_ap = PSB16[bs, 512:512 + 2 * P].ap
                pg.add("dve", lambda v, o=kt.all().ap, i0=k_ap, sc=DKH[h:h + 1].ap: v.tensor_scalar(out=o, in0=i0, scalar1=sc, scalar2=None, op0=ALU.mult),
                       reads=[PSB[bs], DKH[h:h + 1]], writes=[kt.all()])
                sts.append(st_)
                kts.append(kt)
            for h in range(H):
                bo = ps_next()
                for ec in range(4):
                    osl = slice(ec * P, (ec + 1) * P)
                    mm(PSB[bo, osl], V[n, h * DV + ec * P:h * DV + (ec + 1) * P], sts[h].all(), True, False)
                    for dc in range(2):
                        mm(PSB[bo, osl], Sb[2 * h + dc, ec * P:(ec + 1) * P], QD[2 * h + dc, tsl], False, dc == 1)
                sq = SQ.next()
                act(sq.all(), PSB[bo], AF.Square)
                bos.append(bo)
                sqs.append(sq)
            for h in range(H):
                bo, sq = bos[h], sqs[h]
                bgn = ss_next()
                for ec in range(4):
                    mm(PSB[bgn, 0:P], ONES_V.all(), sq[ec * P:(ec + 1) * P], ec == 0, ec == 3)
                rs = RSG_R.next()
                act(rs.all(), PSB[bgn, 0:P], AF.Sqrt, bias=EPSC.all())
                vrecip(rs.all(), rs.all())
                rs_b = rs.all().ap.unsqueeze(1).to_broadcast([P, 4, P])
                o3 = PSB[bo].ap.rearrange("p (a b) -> p a b", a=4)
                yr3 = YR[4 * h:4 * h + 4, tsl]
                pg.add("dve", lambda v, yr3=yr3, o3=o3, rs_b=rs_b: v.tensor_tensor(out=yr3.ap, in0=o3, in1=rs_b, op=ALU.mult),
                       reads=[PSB[bo], rs.all()], writes=[yr3])
            for h in range(H):
                for dc in range(2):
                    bd = ps_next()
                    mm(PSB[bd], kts[h][dc * P:(dc + 1) * P], V[n, h * DV:(h + 1) * DV], True, True)
                    stt(S[2 * h + dc], S[2 * h + dc], GAMMA_C[h], PSB[bd], ALU.mult, ALU.add)
                    act(Sb[2 * h + dc], S[2 * h + dc], AF.Copy)
        pg.tag = 'ret.g'
        for gb in range(H * DV // 256):
            w_t = wload(wview(w_rin, 0, goff + gb * 256, goff + (gb + 1) * 256), [KC, 256], ('rg', gb))
            for hf in range(2):
                c = 2 * gb + hf
                bg = ps_next()
                for k in range(KC):
                    mm(PSB[bg], w_t[k, hf * P:(hf + 1) * P], XN[k], k == 0, k == KC - 1)
                sgt = TMPB.next()
                act(sgt.all(), PSB[bg], AF.Silu)
                tt(YR[c], YR[c], sgt.all(), ALU.mult)
        pg.tag = 'ret.out'
        pn = PostNorm(l, MIX_POST, False)
        for j2 in range(KC // 2):
            o_t = wload(wview(w_rout, 0, j2 * 256, (j2 + 1) * 256), [H * 4, 256], ('ro', j2))
            for hf in range(2):
                j = 2 * j2 + hf
                by = ps_next()
                for c in range(H * 4):
                    mm(PSB[by], o_t[c, hf * P:(hf + 1) * P], YR[c], c == 0, c == H * 4 - 1)
                pn.add_from_psum(j, PSB[by])
        pn.finish()

    def ple(l):
        pg.tag = 'ple'
        prenorm(l, PLE_PRE)
        pg.tag = 'ple.main'
        pn = PostNorm(l, PLE_POST, False)
        for j2 in range(KC // 2):
            g_t = wload(wview(w_pg, l, j2 * 256, (j2 + 1) * 256), [KC, 256], ('pg', l, j2))
            p_t = wload(wview(w_pp, l, j2 * 256, (j2 + 1) * 256), [2, 256], ('pp', l, j2))
            for hf in range(2):
                j = 2 * j2 + hf
                bg, be = ps_next(), ps_next()
                for k in range(KC):
                    mm(PSB[bg], g_t[k, hf * P:(hf + 1) * P], XN[k], k == 0, k == KC - 1)
                for k in range(2):
                    mm(PSB[be], p_t[k, hf * P:(hf + 1) * P], PT[l, k], k == 0, k == 1)
                sg = TMPB.next()
                act(sg.all(), PSB[bg], AF.Sigmoid)
                tt(Y[j], sg.all(), PSB[be], ALU.mult)
                pn.add_from_y(j)
        pn.finish()

    out_dmas = []

    def load_tile(ti):
        pg.tag = 'load'
        t0 = ti * TT
        for n in range(NCH):
            dma_in("sp", ISLOT[n].all(), x_d[t0 + n * P:t0 + (n + 1) * P, :])
        for n in range(NCH):
            xs = ISLOT[n]
            for hb in range(2):
                b = ps_next()
                for q in range(4):
                    c = hb * 4 + q
                    tr(PSB[b, q * P:(q + 1) * P], xs[c * P:(c + 1) * P], IDF.all())
                dst = X[hb * 4:hb * 4 + 4, n * P:(n + 1) * P]
                src = PSB[b].ap.rearrange("p (a b) -> p a b", a=4)
                pg.add("act", lambda a, dst=dst, src=src: a.activation(dst.ap, src, AF.Copy),
                       reads=[PSB[b]], writes=[dst])
            for l in layers:
                pst = PS_ST.next()
                dma_in("sp", pst.all(), p_d[l, t0 + n * P:t0 + (n + 1) * P, :])
                b = ps_next()
                for k in range(2):
                    tr(PSB[b, k * P:(k + 1) * P], pst[k * P:(k + 1) * P], IDF.all())
                dst = PT[l, 0:2, n * P:(n + 1) * P]
                src = PSB[b, 0:2 * P].ap.rearrange("p (a b) -> p a b", a=2)
                pg.add("dve", lambda v, dst=dst, src=src: v.tensor_copy(out=dst.ap, in_=src),
                       reads=[PSB[b, 0:2 * P]], writes=[dst])
        if 1 in layers:
            dma_in("sp", CS[0], c_cos[:, t0:t0 + TT])
            dma_in("sp", CS[1], c_sin[:, t0:t0 + TT])

    def store_tile(ti):
        pg.tag = 'store'
        t0 = ti * TT
        for n in range(NCH):
            xs = OSLOT[n]
            for hb in range(2):
                b = ps_next()
                for q in range(4):
                    c = hb * 4 + q
                    tr(PSB[b, q * P:(q + 1) * P], X[c, n * P:(n + 1) * P], IDF.all())
                act(xs[hb * 4 * P:(hb * 4 + 4) * P], PSB[b], AF.Copy)
            out_dmas.append(dma_out(y_d[t0 + n * P:t0 + (n + 1) * P, :], xs.all()))

    for ti in range(ntile):
        cur_tile[0] = ti
        load_tile(ti)
        for l in layers:
            ffn(l, w_f1g, w_f1u, w_f1d, FFN1_PRE, FFN1_POST, 'f1')
            if l % 2 == 0:
                conv_mixer(l)
            else:
                retention(l)
            ffn(l, w_f2g, w_f2u, w_f2d, FFN2_PRE, FFN2_POST, 'f2')
            ple(l)
        store_tile(ti)
    fin = pg.add("sp", None)
    fin.deps.update(out_dmas)

    pg.finalize()

    sem_cms = []
    sems = {}

    def mksem(key, name):
        cm = nc.semaphore(name)
        sem_cms.append(cm)
        sems[key] = cm.__enter__()

    for e in ("pe", "act", "dve", "pool", "sp"):
        mksem((e, "c"), "c_" + e)
    for e in ("pool", "sp", "act"):
        for s_ in range(Prog.NDMA_SEM):
            mksem((e, s_), "d_%s_%d" % (e, s_))

    with nc.Block() as block:
        @block.tensor
        def _(t):
            pg.emit_engine("pe", t, sems)

        @block.scalar
        def _(a):
            pg.emit_engine("act", a, sems)

        @block.vector
        def _(v):
            pg.emit_engine("dve", v, sems)

        @block.gpsimd
        def _(g):
            pg.emit_engine("pool", g, sems)

        @block.sync
        def _(s):
            pg.emit_engine("sp", s, sems)

    for cm in reversed(sem_cms):
        cm.__exit__(None, None, None)
    psum_cm.__exit__(None, None, None)
    arena_cm.__exit__(None, None, None)
    nstat = {e: len(pg.ops[e]) for e in Prog.ENGS}
    nstat["_pe_tags"] = [op.tag for op in pg.ops["pe"]]
    return nc, nstat


def make_consts(T):
    f32 = np.float32
    ident = np.eye(P, dtype=f32)
    gam = np.array([1.0 - 2.0 ** (-5 - h) for h in range(H)], dtype=np.float64)
    idx = np.arange(CH, dtype=np.float64)
    diff = idx[None, :] - idx[:, None]
    mask = np.zeros((P, H, P), dtype=f32)
    for h in range(H):
        m = np.where(diff >= 0, gam[h] ** np.maximum(diff, 0.0), 0.0) * (DK ** -0.5)
        mask[:, h, :] = m.astype(f32)
    dq = np.zeros((P, H, P), dtype=f32)
    for h in range(H):
        row = gam[h] ** (idx + 1.0)
        dq[:, h, :] = row.astype(f32)[None, :]
    dk = np.zeros((P, H), dtype=f32)
    for h in range(H):
        dk[:, h] = (gam[h] ** (CH - 1.0 - idx) * (DK ** -0.5)).astype(f32)
    half = DK // 2
    inv_freq = (1.0 / (np.float32(10000.0) ** np.linspace(0.0, 1.0, half, dtype=f32))).astype(f32)
    pos = np.arange(T, dtype=f32)
    ang = (inv_freq[:, None] * pos[None, :]).astype(f32)
    return {
        "c_ident": ident, "c_mask": mask, "c_dq": dq, "c_dk": dk,
        "c_cos": np.cos(ang).astype(f32), "c_sin": np.sin(ang).astype(f32),
    }


_WNAMES = ["norm_g", "ffn1_w_gate", "ffn1_w_up", "ffn1_w_down", "ffn2_w_gate", "ffn2_w_up", "ffn2_w_down",
           "conv_w_in", "conv_w", "conv_w_out", "ret_w_in", "ret_w_out", "ple_w_proj", "ple_w_gate"]


def run(inputs, T, n_cores, layers=(0, 1), trace=False):
    nc, _ = build(T, layers)
    consts = make_consts(T)
    shared = {k: np.ascontiguousarray(np.asarray(inputs[k], dtype=np.float32)) for k in _WNAMES}
    shared.update(consts)
    x = np.asarray(inputs["x"], dtype=np.float32)
    p = np.asarray(inputs["p"], dtype=np.float32)
    in_maps = []
    for c in range(n_cores):
        m = dict(shared)
        m["x"] = np.ascontiguousarray(x[c])
        m["p"] = np.ascontiguousarray(p[:, c])
        in_maps.append(m)
    res = run_bass_kernel_spmd(nc, in_maps, core_ids=list(range(n_cores)), trace=trace)
    out = np.stack([np.asarray(r["y"]) for r in res.results], axis=0)
    return out, res


def kernel(**inputs):
    out, _ = run(inputs, SEQ, N_CORES)
    return out.astype(np.float32)
```
